# Optimizing a Trainium2 kernel written in Bass

```python
import math
import jax, jax.numpy as jnp
from jax import lax
import numpy as np

D_MODEL = 2048
BATCH = 4
SEQ = 4096
DEPTH = 4

DN_ALPHA = (2 * DEPTH) ** 0.25
DN_BETA = (8 * DEPTH) ** -0.25
LN_EPS = 1e-5

A_HEADS = 8
A_HEAD_DIM = 128
A_WIDTH = A_HEADS * A_HEAD_DIM
MOBA_BLOCK = 256
MOBA_TOPK = 3
MOBA_QCHUNK = 16
ROPE_THETA = 500000.0
ROPE_DIM = A_HEAD_DIM // 4
NEG_INF = -1e30

B_HEADS = 4
B_HEAD_DIM = 256
B_WIDTH = B_HEADS * B_HEAD_DIM
MLSTM_CHUNK = 64
B_CONV = 4
HNORM_EPS = 1e-6

EVEN_IN = 3 * A_WIDTH + 4 * B_WIDTH + 2 * B_HEADS
EVEN_SPLITS = (A_WIDTH, 2 * A_WIDTH, 3 * A_WIDTH, 3 * A_WIDTH + 2 * B_WIDTH,
               3 * A_WIDTH + 3 * B_WIDTH, 3 * A_WIDTH + 4 * B_WIDTH)
EVEN_MIX = A_WIDTH + B_WIDTH

C_HEAD_DIM = 64
C_HEADS = D_MODEL // C_HEAD_DIM
LORA_W = 96
LORA_A = 96
LORA_V = 64
LORA_G = 256
LNX_EPS = 64e-5
KK_EPS = 1e-12

N_GROUPS = 4
EXPERTS_PER_GROUP = 8
N_EXPERTS = N_GROUPS * EXPERTS_PER_GROUP
MOE_TOPK = 2
EXPERT_FF = 512
MOE_BLOCK = 128

N_EVEN = (DEPTH + 1) // 2
N_ODD = DEPTH // 2
N_VRES = max(N_ODD - 1, 0)

kernel_name = "moba_mlstm_rwkv7_hmoe_deepnorm_trunk"

F32 = jnp.float32


def layer_norm(x, g, b):
    xf = x.astype(F32)
    mu = jnp.mean(xf, -1, keepdims=True)
    var = jnp.mean(jnp.square(xf - mu), -1, keepdims=True)
    return ((xf - mu) * lax.rsqrt(var + LN_EPS) * g.astype(F32) + b.astype(F32)).astype(x.dtype)


def split_heads(t, n_heads):
    bsz, s, w = t.shape
    return t.reshape(bsz, s, n_heads, w // n_heads).transpose(0, 2, 1, 3)


def merge_heads(t):
    bsz, h, s, d = t.shape
    return t.transpose(0, 2, 1, 3).reshape(bsz, s, h * d)


def partial_rotary(x, pos):
    half = ROPE_DIM // 2
    inv = jnp.power(jnp.float32(ROPE_THETA), -jnp.arange(half, dtype=F32) / half)
    ang = pos.astype(F32)[:, None] * inv[None, :]
    cos, sin = jnp.cos(ang), jnp.sin(ang)
    xr = x[..., :ROPE_DIM].astype(F32)
    x1, x2 = xr[..., :half], xr[..., half:]
    rot = jnp.concatenate([x1 * cos - x2 * sin, x2 * cos + x1 * sin], -1)
    return jnp.concatenate([rot.astype(x.dtype), x[..., ROPE_DIM:]], -1)


def moba_attention(q, k, v):
    bsz, h, s, dh = q.shape
    nblk = s // MOBA_BLOCK
    n_sel = min(MOBA_TOPK, nblk)
    kb = k.reshape(bsz, h, nblk, MOBA_BLOCK, dh)
    vb = v.reshape(bsz, h, nblk, MOBA_BLOCK, dh)
    kmean = jnp.mean(kb.astype(F32), axis=3)
    gate = jnp.einsum('bhsd,bhnd->bhsn', q.astype(F32), kmean)
    qblk = jnp.arange(s) // MOBA_BLOCK
    past = jnp.arange(nblk)[None, :] < qblk[:, None]
    gate = jnp.where(past, gate, -jnp.inf)
    _, sel = lax.top_k(gate, n_sel)
    sel_valid = sel < qblk[:, None]
    scale = A_HEAD_DIM ** -0.5
    nq = s // MOBA_QCHUNK

    def to_chunks(t):
        t = t.reshape(bsz, h, nq, MOBA_QCHUNK, *t.shape[3:])
        return jnp.moveaxis(t, 2, 0)

    bi = jnp.arange(bsz)[:, None, None, None]
    hi = jnp.arange(h)[None, :, None, None]

    def attend(args):
        qc, selc, validc, c = args
        q0 = c * MOBA_QCHUNK
        own = q0 // MOBA_BLOCK
        qpos = q0 + jnp.arange(MOBA_QCHUNK)
        kpos = own * MOBA_BLOCK + jnp.arange(MOBA_BLOCK)
        k_own = lax.dynamic_index_in_dim(kb, own, axis=2, keepdims=False)
        v_own = lax.dynamic_index_in_dim(vb, own, axis=2, keepdims=False)
        s_own = jnp.einsum('bhqd,bhkd->bhqk', qc, k_own).astype(F32) * scale
        s_own = jnp.where(kpos[None, :] <= qpos[:, None], s_own, NEG_INF)
        k_sel = kb[bi, hi, selc]
        v_sel = vb[bi, hi, selc]
        s_sel = jnp.einsum('bhqd,bhqnkd->bhqnk', qc, k_sel).astype(F32) * scale
        s_sel = jnp.where(validc[..., None], s_sel, NEG_INF)
        logits = jnp.concatenate(
            [s_own, s_sel.reshape(bsz, h, MOBA_QCHUNK, n_sel * MOBA_BLOCK)], -1)
        p = jax.nn.softmax(logits, axis=-1).astype(v.dtype)
        p_own = p[..., :MOBA_BLOCK]
        p_sel = p[..., MOBA_BLOCK:].reshape(bsz, h, MOBA_QCHUNK, n_sel, MOBA_BLOCK)
        return (jnp.einsum('bhqk,bhkd->bhqd', p_own, v_own)
                + jnp.einsum('bhqnk,bhqnkd->bhqd', p_sel, v_sel))

    out = lax.map(attend, (to_chunks(q), to_chunks(sel), to_chunks(sel_valid), jnp.arange(nq)))
    return jnp.moveaxis(out, 0, 2).reshape(bsz, h, s, dh)


def mlstm_chunkwise(q, k, v, i_pre, f_pre):
    bsz, h, s, dk = q.shape
    dv = v.shape[-1]
    L = MLSTM_CHUNK
    nc = s // L
    q = q.astype(F32)
    k = k.astype(F32) * (dk ** -0.5)
    v = v.astype(F32)
    log_f = jax.nn.log_sigmoid(f_pre.astype(F32))
    log_i = i_pre.astype(F32)

    def chunks(t):
        t = t.reshape(bsz, h, nc, L, *t.shape[3:])
        return jnp.moveaxis(t, 2, 0)

    tri = jnp.tril(jnp.ones((L, L), dtype=bool))

    def step(carry, xs):
        C, n, m = carry
        qc, kc, vc, ic, fc = xs
        b = jnp.cumsum(fc, axis=-1)
        log_inter = b + m[..., None]
        log_intra = b[..., :, None] - b[..., None, :] + ic[..., None, :]
        log_intra = jnp.where(tri, log_intra, -jnp.inf)
        m_t = jnp.maximum(log_inter, jnp.max(log_intra, -1))
        w_intra = jnp.exp(log_intra - m_t[..., None])
        w_inter = jnp.exp(log_inter - m_t)
        qk = jnp.einsum('bhtd,bhsd->bhts', qc, kc) * w_intra
        num = (w_inter[..., None] * jnp.einsum('bhtd,bhde->bhte', qc, C)
               + jnp.einsum('bhts,bhse->bhte', qk, vc))
        den = w_inter * jnp.einsum('bhtd,bhd->bht', qc, n) + jnp.sum(qk, -1)
        h_out = num / jnp.maximum(jnp.abs(den), jnp.exp(-m_t))[..., None]
        b_last = b[..., -1]
        log_s = b_last[..., None] - b + ic
        m_new = jnp.maximum(b_last + m, jnp.max(log_s, -1))
        carry_decay = jnp.exp(b_last + m - m_new)
        ws = jnp.exp(log_s - m_new[..., None])
        kw = kc * ws[..., None]
        C_new = carry_decay[..., None, None] * C + jnp.einsum('bhsd,bhse->bhde', kw, vc)
        n_new = carry_decay[..., None] * n + jnp.sum(kw, axis=2)
        return (C_new, n_new, m_new), h_out

    init = (jnp.zeros((bsz, h, dk, dv), F32), jnp.zeros((bsz, h, dk), F32),
            jnp.zeros((bsz, h), F32))
    _, hs = lax.scan(step, init, (chunks(q), chunks(k), chunks(v), chunks(log_i), chunks(log_f)))
    return jnp.moveaxis(hs, 0, 2).reshape(bsz, h, s, dv)


def causal_depthwise_conv(u, w, b):
    kw = w.shape[0]
    s = u.shape[1]
    up = jnp.pad(u, ((0, 0), (kw - 1, 0), (0, 0)))
    out = b
    for j in range(kw):
        out = out + up[:, j:j + s] * w[j]
    return out


def even_mixer(x, w_in, gate_bias, conv_w, conv_b, hnorm_g, w_out):
    bsz, s, _ = x.shape
    z = jnp.einsum('bsd,de->bse', x, w_in)
    q_a, k_a, v_a, qk_b, v_b, o_b, gates = jnp.split(z, list(EVEN_SPLITS), axis=-1)
    pos = jnp.arange(s)
    q_a = partial_rotary(split_heads(q_a, A_HEADS), pos)
    k_a = partial_rotary(split_heads(k_a, A_HEADS), pos)
    v_a = split_heads(v_a, A_HEADS)
    s_pad = -(-s // MOBA_BLOCK) * MOBA_BLOCK
    padw = ((0, 0), (0, 0), (0, s_pad - s), (0, 0))
    y_a = moba_attention(jnp.pad(q_a, padw), jnp.pad(k_a, padw), jnp.pad(v_a, padw))[:, :, :s]
    y_a = merge_heads(y_a).astype(x.dtype)
    qk_b = jax.nn.silu(causal_depthwise_conv(qk_b, conv_w, conv_b))
    q_b, k_b = jnp.split(qk_b, 2, axis=-1)
    gates = gates.astype(F32) + gate_bias.astype(F32).reshape(2 * B_HEADS)
    i_pre = gates[..., :B_HEADS].transpose(0, 2, 1)
    f_pre = gates[..., B_HEADS:].transpose(0, 2, 1)
    hb = mlstm_chunkwise(split_heads(q_b, B_HEADS), split_heads(k_b, B_HEADS),
                         split_heads(v_b, B_HEADS), i_pre, f_pre)
    hb = hb * lax.rsqrt(jnp.mean(hb * hb, -1, keepdims=True) + HNORM_EPS)
    y_b = merge_heads(hb).astype(x.dtype) * hnorm_g * jax.nn.sigmoid(o_b)
    y = jnp.concatenate([y_a, y_b], axis=-1)
    return jnp.einsum('bse,ed->bsd', y, w_out)


def rwkv7_mixer(x, v_first, mu, w_r, w_k, w_v, w_o, w0, w1, w2, a0, a1, a2, g1, g2,
                k_k, k_a, r_k, lnx_g, lnx_b, vres):
    bsz, s, d = x.shape
    dx = jnp.pad(x, ((0, 0), (1, 0), (0, 0)))[:, :s] - x
    xr, xw, xk, xv, xa, xg = (x + dx * mu[j] for j in range(6))
    r = xr @ w_r
    k = xk @ w_k
    v = xv @ w_v
    log_w = -jax.nn.softplus(-(w0 + jnp.tanh(xw @ w1) @ w2).astype(F32)) - 0.5
    decay = jnp.exp(-jnp.exp(log_w))
    a = jax.nn.sigmoid((a0 + (xa @ a1) @ a2).astype(F32))
    g = jax.nn.sigmoid(xg @ g1) @ g2
    if vres is None:
        v_first = v
    else:
        v0, v1, v2 = vres
        v = v + (v_first - v) * jax.nn.sigmoid(v0 + (xv @ v1) @ v2)

    def heads(t):
        return t.astype(F32).reshape(bsz, s, C_HEADS, C_HEAD_DIM)

    hd = (C_HEADS, C_HEAD_DIM)
    rh, vh, ah, wh = heads(r), heads(v), heads(a), heads(decay)
    kk = heads(k) * k_k.astype(F32).reshape(hd)
    kk = kk / jnp.maximum(jnp.sqrt(jnp.sum(kk * kk, -1, keepdims=True)), KK_EPS)
    kh = heads(k) * (1.0 + (ah - 1.0) * k_a.astype(F32).reshape(hd))

    def tm(t):
        return jnp.moveaxis(t, 1, 0)

    def step(state, xs):
        r_t, w_t, k_t, v_t, kk_t, a_t = xs
        u = jnp.einsum('bhvk,bhk->bhv', state, kk_t)
        state = (state * w_t[:, :, None, :] - u[..., None] * (kk_t * a_t)[:, :, None, :]
                 + v_t[..., None] * k_t[:, :, None, :])
        return state, jnp.einsum('bhvk,bhk->bhv', state, r_t)

    s0 = jnp.zeros((bsz, C_HEADS, C_HEAD_DIM, C_HEAD_DIM), F32)
    _, y = lax.scan(step, s0, (tm(rh), tm(wh), tm(kh), tm(vh), tm(kk), tm(ah)))
    y = jnp.moveaxis(y, 0, 1)
    ym = jnp.mean(y, -1, keepdims=True)
    yv = jnp.mean(jnp.square(y - ym), -1, keepdims=True)
    y = ((y - ym) * lax.rsqrt(yv + LNX_EPS)).reshape(bsz, s, d) * lnx_g.astype(F32) + lnx_b.astype(F32)
    bonus = jnp.sum(rh * kh * r_k.astype(F32).reshape(hd), -1, keepdims=True) * vh
    y = (y + bonus.reshape(bsz, s, d)).astype(x.dtype) * g
    return y @ w_o, v_first


def grouped_expert_mlp(xf, expert_idx, w_gate, w_up, w_down):
    t, kk = expert_idx.shape
    d = xf.shape[-1]
    m = t * kk
    e = w_gate.shape[0]
    flat_e = expert_idx.reshape(m)
    order = jnp.argsort(flat_e)
    sorted_e = flat_e[order]
    counts = jnp.bincount(flat_e, length=e)
    padded = (counts + MOE_BLOCK - 1) // MOE_BLOCK * MOE_BLOCK
    pad_end = jnp.cumsum(padded)
    pad_start = pad_end - padded
    start = jnp.cumsum(counts) - counts
    dest = pad_start[sorted_e] + jnp.arange(m) - start[sorted_e]
    n_blocks = (m + e * (MOE_BLOCK - 1) + MOE_BLOCK - 1) // MOE_BLOCK
    buf = jnp.zeros((n_blocks * MOE_BLOCK, d), xf.dtype).at[dest].set(xf[order // kk])
    block_e = jnp.minimum(
        jnp.searchsorted(pad_end, jnp.arange(n_blocks) * MOE_BLOCK, side='right'), e - 1)

    def expert_block(args):
        xb, ei = args
        hid = jax.nn.silu(xb @ w_gate[ei]) * (xb @ w_up[ei])
        return hid @ w_down[ei]

    yb = lax.map(expert_block, (buf.reshape(n_blocks, MOE_BLOCK, d), block_e))
    y_sorted = yb.reshape(-1, d)[dest]
    return jnp.zeros((m, d), yb.dtype).at[order].set(y_sorted).reshape(t, kk, d)


def hierarchical_moe(x, w_grp, b_grp, w_exp_r, b_exp_r, w_gate, w_up, w_down):
    bsz, s, d = x.shape
    xf = x.reshape(-1, d)
    t = xf.shape[0]
    g_prob = jax.nn.softmax((xf @ w_grp + b_grp).astype(F32), axis=-1)
    g_p, g_idx = lax.top_k(g_prob, 1)
    e_logits = (xf @ w_exp_r + b_exp_r).astype(F32).reshape(t, N_GROUPS, EXPERTS_PER_GROUP)
    e_logits = jnp.take_along_axis(e_logits, g_idx[:, :, None], axis=1)[:, 0]
    e_p, e_idx = lax.top_k(jax.nn.softmax(e_logits, axis=-1), MOE_TOPK)
    e_p = e_p / jnp.sum(e_p, -1, keepdims=True)
    gates = g_p * e_p
    expert = g_idx * EXPERTS_PER_GROUP + e_idx
    y = grouped_expert_mlp(xf, expert, w_gate, w_up, w_down)
    out = jnp.einsum('tk,tkd->td', gates.astype(y.dtype), y)
    return out.reshape(bsz, s, d)


def setup_inputs(seed: int = 0) -> dict:
    key = jax.random.key(seed)
    keys = iter(jax.random.split(key, 48))
    D = D_MODEL

    def nrm(shape, scale):
        return jax.random.normal(next(keys), shape, F32) * scale

    inp = {}
    inp['x'] = nrm((BATCH, SEQ, D), 1.0)
    inp['ev_w_in'] = nrm((N_EVEN, D, EVEN_IN), D ** -0.5)
    inp['ev_gate_bias'] = jnp.stack([nrm((N_EVEN, B_HEADS), 0.1),
                                     3.0 + nrm((N_EVEN, B_HEADS), 0.5)], axis=1)
    inp['ev_conv_w'] = nrm((N_EVEN, B_CONV, 2 * B_WIDTH), 0.5)
    inp['ev_conv_b'] = nrm((N_EVEN, 2 * B_WIDTH), 0.02)
    inp['ev_hnorm_g'] = 1.0 + nrm((N_EVEN, B_WIDTH), 0.05)
    inp['ev_w_out'] = nrm((N_EVEN, EVEN_MIX, D), EVEN_MIX ** -0.5 * DN_BETA)
    inp['od_mu'] = jax.random.uniform(next(keys), (N_ODD, 6, D), F32)
    inp['od_w_r'] = nrm((N_ODD, D, D), D ** -0.5)
    inp['od_w_k'] = nrm((N_ODD, D, D), D ** -0.5)
    inp['od_w_v'] = nrm((N_ODD, D, D), D ** -0.5)
    inp['od_w_o'] = nrm((N_ODD, D, D), D ** -0.5 * DN_BETA)
    inp['od_w0'] = -2.0 + nrm((N_ODD, D), 1.0)
    inp['od_w1'] = nrm((N_ODD, D, LORA_W), D ** -0.5)
    inp['od_w2'] = nrm((N_ODD, LORA_W, D), 0.1 * LORA_W ** -0.5)
    inp['od_a0'] = nrm((N_ODD, D), 0.1)
    inp['od_a1'] = nrm((N_ODD, D, LORA_A), D ** -0.5)
    inp['od_a2'] = nrm((N_ODD, LORA_A, D), 0.1 * LORA_A ** -0.5)
    inp['od_g1'] = nrm((N_ODD, D, LORA_G), D ** -0.5)
    inp['od_g2'] = nrm((N_ODD, LORA_G, D), LORA_G ** -0.5)
    inp['od_k_k'] = 0.85 + nrm((N_ODD, D), 0.05)
    inp['od_k_a'] = 1.0 + nrm((N_ODD, D), 0.05)
    inp['od_r_k'] = nrm((N_ODD, D), 0.1)
    inp['od_lnx_g'] = 1.0 + nrm((N_ODD, D), 0.05)
    inp['od_lnx_b'] = nrm((N_ODD, D), 0.02)
    inp['od_v0'] = 1.0 + nrm((N_VRES, D), 0.1)
    inp['od_v1'] = nrm((N_VRES, D, LORA_V), D ** -0.5)
    inp['od_v2'] = nrm((N_VRES, LORA_V, D), 0.1 * LORA_V ** -0.5)
    inp['ln_mix_g'] = 1.0 + nrm((DEPTH, D), 0.05)
    inp['ln_mix_b'] = nrm((DEPTH, D), 0.02)
    inp['ln_ffn_g'] = 1.0 + nrm((DEPTH, D), 0.05)
    inp['ln_ffn_b'] = nrm((DEPTH, D), 0.02)
    inp['moe_w_grp'] = nrm((DEPTH, D, N_GROUPS), D ** -0.5)
    inp['moe_b_grp'] = nrm((DEPTH, N_GROUPS), 0.01)
    inp['moe_w_exp_r'] = nrm((DEPTH, D, N_EXPERTS), D ** -0.5)
    inp['moe_b_exp_r'] = nrm((DEPTH, N_EXPERTS), 0.01)
    inp['moe_w_gate'] = nrm((DEPTH, N_EXPERTS, D, EXPERT_FF), D ** -0.5)
    inp['moe_w_up'] = nrm((DEPTH, N_EXPERTS, D, EXPERT_FF), D ** -0.5)
    inp['moe_w_down'] = nrm((DEPTH, N_EXPERTS, EXPERT_FF, D), EXPERT_FF ** -0.5 * DN_BETA)
    return inp


def reference(x, ev_w_in, ev_gate_bias, ev_conv_w, ev_conv_b, ev_hnorm_g, ev_w_out,
              od_mu, od_w_r, od_w_k, od_w_v, od_w_o, od_w0, od_w1, od_w2, od_a0, od_a1, od_a2,
              od_g1, od_g2, od_k_k, od_k_a, od_r_k, od_lnx_g, od_lnx_b, od_v0, od_v1, od_v2,
              ln_mix_g, ln_mix_b, ln_ffn_g, ln_ffn_b,
              moe_w_grp, moe_b_grp, moe_w_exp_r, moe_b_exp_r, moe_w_gate, moe_w_up, moe_w_down):
    v_first = None
    for layer in range(DEPTH):
        if layer % 2 == 0:
            e = layer // 2
            mix = even_mixer(x, ev_w_in[e], ev_gate_bias[e], ev_conv_w[e], ev_conv_b[e],
                             ev_hnorm_g[e], ev_w_out[e])
        else:
            o = layer // 2
            vres = None if o == 0 else (od_v0[o - 1], od_v1[o - 1], od_v2[o - 1])
            mix, v_first = rwkv7_mixer(x, v_first, od_mu[o], od_w_r[o], od_w_k[o], od_w_v[o],
                                       od_w_o[o], od_w0[o], od_w1[o], od_w2[o], od_a0[o],
                                       od_a1[o], od_a2[o], od_g1[o], od_g2[o], od_k_k[o],
                                       od_k_a[o], od_r_k[o], od_lnx_g[o], od_lnx_b[o], vres)
        x = layer_norm(DN_ALPHA * x + mix, ln_mix_g[layer], ln_mix_b[layer])
        ffn = hierarchical_moe(x, moe_w_grp[layer], moe_b_grp[layer], moe_w_exp_r[layer],
                               moe_b_exp_r[layer], moe_w_gate[layer], moe_w_up[layer],
                               moe_w_down[layer])
        x = layer_norm(DN_ALPHA * x + ffn, ln_ffn_g[layer], ln_ffn_b[layer])
    return x
```

```python
import numpy as np
import concourse.bass as bass
import concourse.mybir as mybir
from concourse.bass_utils import run_bass_kernel_spmd
from concourse.alu_op_type import AluOpType as ALU

F32 = mybir.dt.float32
F32R = mybir.dt.float32r
U32 = mybir.dt.uint32
AF = mybir.ActivationFunctionType
AX = mybir.AxisListType

NCORES = 8
N_DMA_SEMS = 24


class Trk:
    __slots__ = ("lw", "rd", "excl")

    def __init__(self, excl=False):
        self.lw = None
        self.rd = []
        self.excl = excl


class V:
    __slots__ = ("ap", "trks")

    def __init__(self, ap, trks):
        self.ap = ap
        self.trks = trks

    def __getitem__(self, k):
        return V(self.ap[k], self.trks)

    def bitcast(self, dt):
        return V(self.ap.bitcast(dt), self.trks)

    def rearrange(self, s, **kw):
        return V(self.ap.rearrange(s, **kw), self.trks)

    def bcast(self, shape):
        return V(self.ap.to_broadcast(shape), self.trks)

    def partition_broadcast(self, n):
        return V(self.ap.partition_broadcast(n), self.trks)

    def bc(self, axis, shape):
        return V(self.ap.unsqueeze(axis).to_broadcast(list(shape)), self.trks)

    @property
    def r(self):
        return V(self.ap.bitcast(F32R), self.trks)


class Buf:
    def __init__(self, t, excl=False):
        self.t = t
        self.trk = Trk(excl)
        self.parts = {}

    def __getitem__(self, k):
        return V(self.t[k], [self.trk])

    def part(self, key, k):
        if key not in self.parts:
            self.parts[key] = Trk()
        return V(self.t[k], [self.parts[key]])

    def whole(self, k=slice(None)):
        return V(self.t[k], [self.trk] + list(self.parts.values()))


class Prog:
    ENG = ("pe", "dve", "act", "pool", "sp")

    def __init__(self):
        self.nc = bass.Bass("TRN2", target_bir_lowering=False)
        self.ops = {e: [] for e in self.ENG}
        self.cnt = {e: 0 for e in self.ENG}
        self.known = {e: {} for e in self.ENG}
        self.stack = None
        self.dma_i = 0
        self.dma_ip = 0
        self.dma_uses = [0] * N_DMA_SEMS
        self.out_events = []
        self.nbuf = 0

    def begin(self, stack):
        self.stack = stack
        nc = self.nc
        self.sems = {}
        for e in ("pe", "dve", "act", "pool"):
            self.sems[e] = stack.enter_context(nc.semaphore("s_" + e))
        for i in range(N_DMA_SEMS):
            self.sems[("d", i)] = stack.enter_context(nc.semaphore("s_d%d" % i))

    def dram_in(self, name, shape, dt=F32):
        return Buf(self.nc.dram_tensor(name, list(shape), dt, kind="ExternalInput").ap())

    def dram_out(self, name, shape, dt=F32):
        return Buf(self.nc.dram_tensor(name, list(shape), dt, kind="ExternalOutput").ap())

    def sb(self, shape, dt=F32, name=None):
        self.nbuf += 1
        t = self.stack.enter_context(self.nc.sbuf_tensor(name or "sb%d" % self.nbuf, list(shape), dt))
        return Buf(t)

    def ps(self, shape=(128, 512), dt=F32, name=None):
        self.nbuf += 1
        t = self.stack.enter_context(self.nc.psum_tensor(name or "ps%d" % self.nbuf, list(shape), dt))
        return Buf(t, excl=True)

    def barrier(self):
        targets = [(e, self.cnt[e]) for e in ("pe", "dve", "act", "pool") if self.cnt[e] > 0]
        targets += [(("d", i), 16 * u) for i, u in enumerate(self.dma_uses) if u > 0]
        sems = self.sems
        for eng in self.ENG:
            kn = self.known[eng]
            waits = []
            for k, v in targets:
                if k == eng or kn.get(k, 0) >= v:
                    continue
                kn[k] = v
                waits.append((k, v))
            if not waits:
                continue

            def emit(e, waits=waits):
                for k, v in waits:
                    e.wait_ge(sems[k], v)
            self.ops[eng].append(emit)

    def scope(self):
        prog = self

        class _Scope:
            def __enter__(s):
                from contextlib import ExitStack
                s.outer = prog.stack
                s.st = ExitStack()
                s.st.__enter__()
                prog.stack = s.st
                return s

            def __exit__(s, *a):
                prog.barrier()
                prog.stack = s.outer
                return s.st.__exit__(*a)
        return _Scope()

    def _deps(self, eng, reads, writes, pe_accum=False):
        deps = {}

        def add(ev):
            if ev is None:
                return
            k, v = ev
            if deps.get(k, 0) < v:
                deps[k] = v
        for r in reads:
            for t in r.trks:
                add(t.lw)
                if t.excl:
                    for ev in t.rd:
                        if ev[0] != eng:
                            add(ev)
        for w in writes:
            for t in w.trks:
                if not (pe_accum and t.lw is not None and t.lw[0] == "pe"):
                    add(t.lw)
                for ev in t.rd:
                    add(ev)
        kn = self.known[eng]
        out = []
        for k, v in deps.items():
            if kn.get(k, 0) >= v:
                continue
            kn[k] = v
            out.append((k, v))
        return out

    def _mark(self, ev, reads, writes):
        for r in reads:
            for t in r.trks:
                t.rd.append(ev)
        for w in writes:
            for t in w.trks:
                t.lw = ev
                t.rd = []

    def op(self, eng, fn, reads, writes, pe_accum=False):
        waits = self._deps(eng, reads, writes, pe_accum)
        self.cnt[eng] += 1
        n = self.cnt[eng]
        sem = self.sems[eng]
        sems = self.sems

        def emit(e):
            for k, v in waits[1:]:
                e.wait_ge(sems[k], v)
            ins = fn(e)
            if waits:
                ins._wait_ge(sems[waits[0][0]], waits[0][1])
            ins.then_inc(sem, 1)
        self.ops[eng].append(emit)
        self._mark((eng, n), reads, writes)

    def _slot(self, q):
        half = N_DMA_SEMS // 2
        if q == "pool":
            i = half + self.dma_ip % half
            self.dma_ip += 1
        else:
            i = self.dma_i % half
            self.dma_i += 1
        return i

    def dma(self, out, in_, q="sp", is_output=False, **kw):
        i = self._slot(q)
        key = ("d", i)
        prev = self.dma_uses[i]
        waits = self._deps(q, [in_], [out])
        if prev > 0 and self.known[q].get(key, 0) < 16 * prev:
            self.known[q][key] = 16 * prev
            waits.append((key, 16 * prev))
        self.dma_uses[i] = prev + 1
        sem = self.sems[key]
        sems = self.sems
        oap, iap = out.ap, in_.ap

        def emit(e):
            for k, v in waits[1:]:
                e.wait_ge(sems[k], v)
            ins = e.dma_start(out=oap, in_=iap, **kw)
            if waits:
                ins._wait_ge(sems[waits[0][0]], waits[0][1])
            ins.then_inc(sem, 16)
        self.ops[q].append(emit)
        ev = (key, 16 * (prev + 1))
        self._mark(ev, [in_], [out])
        if is_output:
            self.out_events.append(ev)

    def allgather(self, out_buf, in_buf):
        i = self._slot("pool")
        key = ("d", i)
        prev = self.dma_uses[i]
        out, in_ = out_buf.whole(), in_buf.whole()
        waits = self._deps("pool", [in_], [out])
        if prev > 0 and self.known["pool"].get(key, 0) < 16 * prev:
            self.known["pool"][key] = 16 * prev
            waits.append((key, 16 * prev))
        self.dma_uses[i] = prev + 1
        sem = self.sems[key]
        sems = self.sems
        oap, iap = out.ap, in_.ap

        def emit(e):
            for k, v in waits:
                e.wait_ge(sems[k], v)
            e.collective_compute("AllGather", ALU.bypass, replica_groups=[list(range(NCORES))],
                                 ins=[iap], outs=[oap]).then_inc(sem, 16)
        self.ops["pool"].append(emit)
        self._mark((key, 16 * (prev + 1)), [in_], [out])

    def pool_dma_custom(self, fn, reads, writes):
        i = self._slot("pool")
        key = ("d", i)
        prev = self.dma_uses[i]
        waits = self._deps("pool", reads, writes)
        if prev > 0 and self.known["pool"].get(key, 0) < 16 * prev:
            self.known["pool"][key] = 16 * prev
            waits.append((key, 16 * prev))
        self.dma_uses[i] = prev + 1
        sem = self.sems[key]
        sems = self.sems

        def emit(e):
            for k, v in waits:
                e.wait_ge(sems[k], v)
            fn(e).then_inc(sem, 16)
        self.ops["pool"].append(emit)
        self._mark((key, 16 * (prev + 1)), reads, writes)

    def scatter_rows(self, out_dram, idx, in_, nrows):
        o, ix, i = out_dram.ap, idx.ap, in_.ap
        self.pool_dma_custom(lambda e: e.indirect_dma_start(
            out=o, out_offset=bass.IndirectOffsetOnAxis(ap=ix, axis=0), in_=i, in_offset=None,
            bounds_check=self._breg(e, nrows - 1), oob_is_err=False), [in_, idx], [out_dram])

    def _breg(self, e, val):
        if not hasattr(self, "_bregs"):
            self._bregs = {}
        if val not in self._bregs:
            self._bregs[val] = e.to_reg(val)
        return self._bregs[val]

    def gather_rows(self, out, in_dram, idx, nrows):
        o, ix, i = out.ap, idx.ap, in_dram.ap

        def fn(e):
            try:
                return e.indirect_dma_start(out=o, out_offset=None, in_=i,
                                            in_offset=bass.IndirectOffsetOnAxis(ap=ix, axis=0),
                                            bounds_check=self._breg(e, nrows - 1), oob_is_err=False)
            except Exception:
                print("GATHER FAIL out", o, "idx", ix, "in", i, flush=True)
                raise
        self.pool_dma_custom(fn, [in_dram, idx], [out])

    def mm(self, out, lhsT, rhs, start=True, stop=True):
        o, l, r = out.ap, lhsT.ap, rhs.ap
        self.op("pe", lambda e: e.matmul(o, l, r, start=start, stop=stop),
                [lhsT, rhs], [out], pe_accum=not start)

    def transpose(self, out, in_, ident):
        o, i, d = out.ap, in_.ap, ident.ap
        self.op("pe", lambda e: e.transpose(o, i, d), [in_, ident], [out])

    def act(self, out, in_, func, bias=None, scale=None, eng="act", accum=None):
        o, i = out.ap, in_.ap
        reads = [in_]
        kw = {}
        if bias is not None:
            if isinstance(bias, V):
                reads.append(bias)
                kw["bias"] = bias.ap
            else:
                kw["bias"] = bias
        if scale is not None:
            if isinstance(scale, V):
                reads.append(scale)
                kw["scale"] = scale.ap
            else:
                kw["scale"] = scale
        writes = [out]
        if accum is not None:
            kw["accum_out"] = accum.ap
            writes.append(accum)
        self.op(eng, lambda e: e.activation(o, i, func, **kw), reads, writes)

    def tt(self, out, a, b, op, eng="dve"):
        o, x, y = out.ap, a.ap, b.ap
        self.op(eng, lambda e: e.tensor_tensor(o, x, y, op), [a, b], [out])

    def ts(self, out, a, s1, op0, s2=None, op1=None, eng="dve", accum=None):
        o, x = out.ap, a.ap
        reads = [a]
        if isinstance(s1, V):
            reads.append(s1)
            s1 = s1.ap
        if isinstance(s2, V):
            reads.append(s2)
            s2 = s2.ap
        writes = [out]
        kw = {}
        if accum is not None:
            kw["accum_out"] = accum.ap
            writes.append(accum)
        if op1 is None:
            self.op(eng, lambda e: e.tensor_scalar(o, x, s1, None, op0, **kw), reads, writes)
        else:
            self.op(eng, lambda e: e.tensor_scalar(o, x, s1, s2, op0, op1, **kw), reads, writes)

    def stt(self, out, a, s, b, op0, op1, eng="dve"):
        o, x, y = out.ap, a.ap, b.ap
        reads = [a, b]
        if isinstance(s, V):
            reads.append(s)
            s = s.ap
        self.op(eng, lambda e: e.scalar_tensor_tensor(o, x, s, y, op0, op1), reads, [out])

    def copy(self, out, in_, eng="dve"):
        o, i = out.ap, in_.ap
        if eng == "act":
            self.op(eng, lambda e: e.copy(o, i), [in_], [out])
        else:
            self.op(eng, lambda e: e.tensor_copy(o, i), [in_], [out])

    def memset(self, out, val, eng="dve"):
        o = out.ap
        self.op(eng, lambda e: e.memset(o, val), [], [out])

    def reduce(self, out, in_, op, axis=None, eng="dve"):
        o, i = out.ap, in_.ap
        ax = axis or AX.X
        self.op(eng, lambda e: e.tensor_reduce(o, i, ax, op), [in_], [out])

    def recip(self, out, in_):
        o, i = out.ap, in_.ap
        self.op("dve", lambda e: e.reciprocal(o, i), [in_], [out])

    def max8(self, out, in_):
        o, i = out.ap, in_.ap
        self.op("dve", lambda e: e.max(o, i), [in_], [out])

    def bn_stats(self, out, in_):
        o, i = out.ap, in_.ap
        self.op("dve", lambda e: e.bn_stats(o, i), [in_], [out])

    def bn_aggr(self, out, in_):
        o, i = out.ap, in_.ap
        self.op("dve", lambda e: e.bn_aggr(o, i), [in_], [out])

    def scan(self, out, d0, d1, init, op0, op1):
        o, a, b = out.ap, d0.ap, d1.ap
        reads = [d0, d1]
        if isinstance(init, V):
            reads.append(init)
            init = init.ap
        self.op("dve", lambda e: e.tensor_tensor_scan(o, a, b, init, op0, op1), reads, [out])

    def finish(self):
        nc = self.nc
        final = {}
        for k, v in self.out_events:
            if final.get(k, 0) < v:
                final[k] = v
        sems = self.sems
        ops = self.ops

        def tail(e):
            for k, v in final.items():
                e.wait_ge(sems[k], v)
        with nc.Block() as block:
            @block.sync
            def _(e):
                for f in ops["sp"]:
                    f(e)
                tail(e)

            @block.tensor
            def _(e):
                for f in ops["pe"]:
                    f(e)

            @block.vector
            def _(e):
                for f in ops["dve"]:
                    f(e)

            @block.scalar
            def _(e):
                for f in ops["act"]:
                    f(e)

            @block.gpsimd
            def _(e):
                for f in ops["pool"]:
                    f(e)
        return nc


D = 2048
SEQ = 4096
DEPTH = 4
DN_ALPHA = 8 ** 0.25
LN_EPS = 1e-5
A_HEADS, A_HD = 8, 128
B_HEADS, B_HD = 4, 256
EVEN_IN = 7176
NEG = -1e30
TB = 1024


def dram_scratch(P, name, shape, dt=F32):
    return Buf(P.nc.dram_tensor(name, list(shape), dt, kind="Internal").ap())


def make_consts():
    c = {}
    c["ident"] = np.eye(128, dtype=np.float32)
    half = 16
    inv = np.power(np.float32(500000.0), -np.arange(half, dtype=np.float32) / half)
    ang = np.arange(SEQ, dtype=np.float32)[:, None] * inv[None, :]
    c["cosE"] = np.tile(np.cos(ang).astype(np.float32), (1, 4))
    c["sinE"] = np.tile(np.sin(ang).astype(np.float32), (1, 4))
    nt, nb = SEQ // 128, SEQ // 256
    qb = np.arange(nt) // 2
    c["moba_pm"] = np.where(np.arange(nb)[None, :] < qb[:, None], 0.0, NEG).astype(np.float32).reshape(-1)
    c["moba_nown"] = (np.arange(nb)[None, :] != qb[:, None]).astype(np.float32).reshape(-1)
    e = np.zeros((16, 16, 128), np.float32)
    for n in range(16):
        e[n, n, :] = 1.0
    c["moba_E"] = e
    kk = np.arange(128)[:, None]
    qq = np.arange(512)[None, :]
    ss, tt_ = np.arange(128)[:, None], np.arange(128)[None, :]
    c["tri_st"] = (tt_ >= ss).astype(np.float32)
    c["slt"] = (ss < tt_).astype(np.float32)
    c["ones128"] = np.ones((128, 128), np.float32)
    c["iota_p"] = np.arange(128, dtype=np.float32).reshape(128, 1)
    c["blk_thr"] = (np.arange(128, dtype=np.float32) * 128.0)
    blk = (ss // 64) == (tt_ // 64)
    c["rw_BT"] = (blk & (ss <= tt_)).astype(np.float32)
    c["rw_BO"] = blk.astype(np.float32)
    c["rw_SU"] = (blk & (ss < tt_)).astype(np.float32)
    c["rw_SL"] = (blk & (ss > tt_)).astype(np.float32)
    c["rw_UI"] = (blk & (ss <= tt_)).astype(np.float32)
    c["moba_cm"] = np.stack([(128 * r + kk <= qq) for r in range(4)], 1).astype(np.float32)
    return c


def load_consts(P, Cd):
    C = dict(Cd)
    ident = P.sb([128, 128], name="ident_sb")
    P.dma(ident[:], Cd["ident"][:])
    C["ident"] = ident
    return C


class XT:
    def __init__(self, P, ident, K, tb=TB):
        self.P, self.K, self.tb = P, K, tb
        self.KT = K // 128
        self.xs = P.sb([128, self.KT, tb], F32R)
        self.xin = [P.sb([128, K]) for _ in range(2)]
        self.tps = [P.ps() for _ in range(2)]
        self.ident = ident
        self.n = 0

    def load(self, x, r0, ntiles, dt=F32R):
        P = self.P
        for i in range(ntiles):
            xi = self.xin[self.n % 2]
            P.dma(xi[:], x[r0 + i * 128: r0 + (i + 1) * 128, :], q="sp" if self.n % 2 else "act")
            gs_ = min(4, self.KT)
            for g in range(self.KT // gs_):
                tp = self.tps[(self.n * (self.KT // gs_) + g) % 2]
                for j in range(gs_):
                    kt = g * gs_ + j
                    P.transpose(tp[:, j * 128:(j + 1) * 128], xi[:, kt * 128:(kt + 1) * 128], self.ident[:])
                P.copy(self.xs[:, g * gs_:(g + 1) * gs_, i * 128:(i + 1) * 128],
                       tp[:, 0:gs_ * 128].rearrange("p (a t) -> p a t", a=gs_), eng="act" if g % 2 else "dve")
            self.n += 1


def lin_stream(P, xt, x, w, T, N, consume, n_lo=0, n_hi=None, wbufs=None, pss=None):
    n_hi = N if n_hi is None else n_hi
    KT = xt.KT
    wbufs = wbufs or [P.sb([128, KT, 512], F32R) for _ in range(2)]
    pss = pss or [P.ps() for _ in range(4)]
    it = 0
    ci = 0
    for b0 in range(0, T, xt.tb):
        nt = min(xt.tb, T - b0) // 128
        xt.load(x, b0, nt)
        for n0 in range(n_lo, n_hi, 512):
            nw = min(512, n_hi - n0)
            wb = wbufs[ci % 2]
            ci += 1
            P.dma(wb[:, :, :nw], w[:, n0:n0 + nw].rearrange("(kt p) n -> p kt n", p=128), q="pool")
            for i in range(nt):
                ps = pss[it % len(pss)]
                it += 1
                for kt in range(KT):
                    P.mm(ps[:, :nw], xt.xs[:, kt, i * 128:(i + 1) * 128], wb[:, kt, :nw],
                         start=(kt == 0), stop=(kt == KT - 1))
                consume(b0 // 128 + i, n0, nw, ps)


def stage_even_inproj(P, C, x, w_in, z, gT, T=SEQ):
    with P.scope():
        xt = XT(P, C["ident"], D)
        ntile = T // 128
        cs = P.sb([128, ntile, 64])
        sn = P.sb([128, ntile, 64])
        P.dma(cs[:], C["cosE"][0:T, :].rearrange("(n p) c -> p n c", p=128))
        P.dma(sn[:], C["sinE"][0:T, :].rearrange("(n p) c -> p n c", p=128), q="act")
        obs = [P.sb([128, 512]) for _ in range(3)]
        tmp = [P.sb([128, 4, 64]) for _ in range(2)]
        wg = P.sb([128, 16, 8], F32R)
        P.dma(wg[:], w_in[:, 7168:7176].rearrange("(kt p) n -> p kt n", p=128), q="pool")
        gps = P.ps()
        gsb = P.sb([8, 512])
        st = {"i": 0}

        def consume(ti, n0, nw, ps):
            k = st["i"]
            st["i"] += 1
            ob = obs[k % 3]
            P.copy(ob[:, :nw], ps[:, :nw], eng="act" if k % 2 else "dve")
            if n0 < 2048:
                o3 = ob[:].rearrange("p (h d) -> p h d", h=4)
                x1, x2 = o3[:, :, 0:16], o3[:, :, 16:32]
                c3 = cs[:, ti, :].rearrange("p (h d) -> p h d", h=4)
                s3 = sn[:, ti, :].rearrange("p (h d) -> p h d", h=4)
                t4 = tmp[k % 2]
                a, b_, c_, d_ = (t4[:, j, :].rearrange("p (h d) -> p h d", h=4) for j in range(4))
                e = "pool" if k % 2 else "dve"
                P.tt(a, x1, c3, ALU.mult, eng=e)
                P.tt(b_, x2, s3, ALU.mult, eng=e)
                P.tt(c_, x2, c3, ALU.mult, eng=e)
                P.tt(d_, x1, s3, ALU.mult, eng=e)
                P.tt(x1, a, b_, ALU.subtract, eng=e)
                P.tt(x2, c_, d_, ALU.add, eng=e)
            P.dma(z[ti * 128:(ti + 1) * 128, n0:n0 + nw], ob[:, :nw], q="sp")

        orig_load = xt.load

        def load_and_gates(xd, r0, nt, **kw):
            orig_load(xd, r0, nt, **kw)
            for h0 in range(0, nt * 128, 512):
                hw = min(512, nt * 128 - h0)
                for kt in range(16):
                    P.mm(gps[0:8, :hw], wg[:, kt, :], xt.xs[:, kt, h0:h0 + hw], start=(kt == 0), stop=(kt == 15))
                P.copy(gsb[:, :hw], gps[0:8, :hw])
                P.dma(gT[:, r0 + h0:r0 + h0 + hw], gsb[:, :hw])
        xt.load = load_and_gates
        lin_stream(P, xt, x, w_in, T, 7168, consume)


def stage_moba(P, C, z, ymix, T=SEQ, upto=3, heads=A_HEADS):
    NT, NBLK = T // 128, T // 256
    scale = A_HD ** -0.5
    with P.scope():
        ident = C["ident"]
        pm = P.sb([128, 32, 16])
        nown = P.sb([128, 32, 16])
        P.dma(pm[:].rearrange("p a b -> p (a b)"), C["moba_pm"][:].partition_broadcast(128))
        P.dma(nown[:].rearrange("p a b -> p (a b)"), C["moba_nown"][:].partition_broadcast(128), q="act")
        Esb = P.sb([128, 16, 128], F32R)
        ez = P.sb([128, 16, 128])
        P.memset(ez[:], 0.0)
        P.copy(Esb[:], ez[:])
        P.dma(Esb[0:16], C["moba_E"][:], q="pool")
        cm = P.sb([128, 4, 512])
        P.dma(cm[:], C["moba_cm"][:])
        qT32 = P.sb([128, T])
        qTr = P.sb([128, T], F32R)
        kTr = P.sb([128, T], F32R)
        vaug = P.sb([128, NT, 130], F32R)
        ones_c = P.sb([128, NT, 2])
        P.memset(ones_c[:], 1.0)
        P.copy(vaug[:, :, 128:130], ones_c[:])
        ksum = P.sb([128, NT])
        kmT = P.sb([128, NBLK])
        biasT = P.sb([128, T], F32R)
        bz = P.sb([128, T])
        P.memset(bz[:], 0.0)
        P.copy(biasT[:], bz[:])
        ld = [P.sb([128, 3, 128]) for _ in range(2)]
        acc = [P.ps() for _ in range(4)]
        sps = [P.ps() for _ in range(2)]
        gps = P.ps()
        tps = P.ps()
        sm = [dict(gm=P.sb([128, 16]), t8=P.sb([128, 8]), b=P.sb([128, 16]), mx=P.sb([128, 8]), m=P.sb([128, 1]))
              for _ in range(2)]
        for S in sm:
            P.memset(S["gm"][:], NEG)
        pTs = [P.sb([128, 512], F32R) for _ in range(3)]
        yo = [P.sb([128, 128]) for _ in range(2)]
        rc = [P.sb([128, 1]) for _ in range(2)]
        for h in range(heads):
            for i in range(NT):
                l = ld[i % 2]
                r = slice(i * 128, (i + 1) * 128)
                src = z[r, :].rearrange("p (a c) -> p a c", c=1024)[:, 0:3, h * 128:(h + 1) * 128]
                P.dma(l[:], src, q="sp" if i % 2 else "act")
                P.transpose(tps[:, 0:128], l[:, 0, :], ident[:])
                P.transpose(tps[:, 128:256], l[:, 1, :], ident[:])
                P.copy(qT32[:, r], tps[:, 0:128], eng="dve")
                P.copy(qTr[:, r], tps[:, 0:128], eng="act")
                P.copy(kTr[:, r], tps[:, 128:256], eng="act")
                P.reduce(ksum[:, i:i + 1], tps[:, 128:256], ALU.add)
                P.copy(vaug[:, i, 0:128], l[:, 2, :], eng="dve")
            k2 = ksum[:].rearrange("p (n two) -> p n two", two=2)
            P.tt(kmT[:], k2[:, :, 0], k2[:, :, 1], ALU.add)
            P.ts(kmT[:], kmT[:], 1.0 / 256.0, ALU.mult)
            if upto < 2:
                continue
            for i in range(NT):
                S = sm[i % 2]
                r = slice(i * 128, (i + 1) * 128)
                P.mm(gps[:, 0:NBLK], qT32[:, r], kmT[:])
                P.tt(S["gm"][:, 0:NBLK], gps[:, 0:NBLK], pm[:, i, 0:NBLK], ALU.add)
                P.max8(S["t8"][:], S["gm"][:])
                P.ts(S["b"][:], S["gm"][:], S["t8"][:, 2:3], ALU.is_ge)
                P.ts(S["b"][:], S["b"][:], 1e30, ALU.mult, -1e30, ALU.add)
                P.tt(S["b"][:], S["b"][:], pm[:, i, :], ALU.add)
                P.tt(S["b"][:], S["b"][:], nown[:, i, :], ALU.mult)
                nch = (i * 128 + 127) // 512 + 1
                for cch in range(nch):
                    sp = sps[cch % 2]
                    P.mm(sp[:], qTr[:, r], kTr[:, cch * 512:(cch + 1) * 512])
                    P.reduce(S["mx"][:, cch:cch + 1], sp[:], ALU.max)
                P.reduce(S["m"][:], S["mx"][:, 0:nch], ALU.max)
                P.ts(S["b"][:], S["b"][:], 1.0 / scale, ALU.mult, S["m"][:], ALU.subtract)
                P.transpose(gps[0:16, 128:256], S["b"][:], ident[:])
                P.copy(biasT[0:16, r], gps[0:16, 128:256], eng="act")
            if upto < 3:
                continue
            k = 0
            for c in range(T // 512):
                q4 = slice(c * 512, (c + 1) * 512)
                last = 4 * c + 3
                for j in range(last + 1):
                    sp = sps[k % 2]
                    pT = pTs[k % 3]
                    k += 1
                    P.mm(sp[:], kTr[:, j * 128:(j + 1) * 128], qTr[:, q4], start=True, stop=False)
                    P.mm(sp[:], Esb[:, j // 2, :], biasT[:, q4], start=False, stop=True)
                    P.act(pT[:], sp[:], AF.Exp, scale=scale)
                    if j >= 4 * c:
                        P.tt(pT[:], pT[:], cm[:, j - 4 * c, :], ALU.mult, eng="pool" if k % 2 else "dve")
                    for t in range(4):
                        if j > 4 * c + t:
                            continue
                        P.mm(acc[t][:, 0:130], pT[:, t * 128:(t + 1) * 128], vaug[:, j, :],
                             start=(j == 0), stop=(j == 4 * c + t))
                for t in range(4):
                    i = 4 * c + t
                    P.recip(rc[t % 2][:], acc[t][:, 128:129])
                    P.ts(yo[t % 2][:], acc[t][:, 0:128], rc[t % 2][:], ALU.mult)
                    P.dma(ymix[i * 128:(i + 1) * 128, h * 128:(h + 1) * 128], yo[t % 2][:])


def stage_mlstm(P, C, z, gT, gate_bias, conv_w, conv_b, hnorm_g, ymix, T=SEQ):
    NC = T // 128
    H, DH = B_HEADS, B_HD
    ident = C["ident"]
    bcol = dram_scratch(P, "ml_bcol_%d" % P.nbuf, [H, 3, 128, NC])
    with P.scope():
        gb = P.sb([1, 8])
        P.dma(gb[:], gate_bias[:].rearrange("(o a) h -> o (a h)", o=1))
        ones_r = P.sb([1, T])
        P.memset(ones_r[:], 1.0)
        one11 = P.sb([1, 2])
        P.memset(one11[:], 1.0)
        ones128 = P.sb([1, 128])
        P.memset(ones128[:], 1.0)
        cps = P.ps()
        ir = P.sb([1, T]); fr = P.sb([1, T]); cl = P.sb([1, T]); a = P.sb([1, T]); A = P.sb([1, T])
        beta = P.sb([1, T]); flo = P.sb([1, T])
        Ap = P.sb([1, NC]); gam = P.sb([1, NC])
        cols = P.sb([128, 3, NC])
        for h in range(H):
            P.dma(ir[:], gT[h:h + 1, :])
            P.dma(fr[:], gT[4 + h:5 + h, :], q="act")
            P.ts(ir[:], ir[:], gb[:, h:h + 1], ALU.add)
            P.ts(fr[:], fr[:], gb[:, 4 + h:5 + h], ALU.add)
            P.act(fr[:], fr[:], AF.Exp, scale=-1.0)
            P.act(fr[:], fr[:], AF.Ln, bias=1.0)
            P.scan(cl[:], ones_r[:], fr[:], 0.0, ALU.mult, ALU.add)
            P.tt(a[:], ir[:], cl[:], ALU.add)
            P.scan(A[:], a[:], a[:], 0.0, ALU.max, ALU.max)
            A3 = A[:].rearrange("o (c l) -> o c l", l=128)
            P.memset(Ap[:], 0.0)
            if NC > 1:
                P.copy(Ap[:, 1:NC], A3[:, 0:NC - 1, 127])
            P.tt(gam[:], Ap[:], A3[:, :, 127], ALU.subtract)
            P.act(gam[:], gam[:], AF.Exp)
            Apb = Ap[:].bc(2, [1, NC, 128])
            P.tt(beta[:].rearrange("o (c l) -> o c l", l=128), a[:].rearrange("o (c l) -> o c l", l=128), Apb, ALU.subtract)
            P.act(beta[:], beta[:], AF.Exp)
            P.ts(beta[:], beta[:], DH ** -0.5, ALU.mult)
            P.tt(flo[:].rearrange("o (c l) -> o c l", l=128), cl[:].rearrange("o (c l) -> o c l", l=128), Apb, ALU.subtract)
            P.act(flo[:], flo[:], AF.Exp)
            for qi, row in enumerate((beta, flo)):
                for c in range(NC):
                    P.mm(cps[:, c:c + 1], row[:, c * 128:(c + 1) * 128], one11[:, 0:1])
                P.copy(cols[:, qi, :], cps[:, 0:NC])
            P.mm(cps[:, 0:NC], ones128[:], gam[:])
            P.copy(cols[:, 2, :], cps[:, 0:NC])
            P.dma(bcol[h].rearrange("q p c -> p q c"), cols[:])
    with P.scope():
        wj = P.sb([128, 4, 2048])
        for j in range(4):
            P.dma(wj[:, j, :], conv_w[j, :].partition_broadcast(128), q="sp" if j % 2 else "act")
        cb = P.sb([128, 2048])
        P.dma(cb[:], conv_b[:].partition_broadcast(128))
        hgb = P.sb([128, 1024])
        P.dma(hgb[:], hnorm_g[:].partition_broadcast(128), q="act")
        cols = P.sb([128, H, 3, NC])
        P.dma(cols[:], bcol[:].rearrange("h q p c -> p h q c"))
        tri = P.sb([128, 128])
        P.dma(tri[:], C["tri_st"][:])
        C32 = P.sb([128, H, 2, 258])
        P.memset(C32[:], 0.0)
        Cr = P.sb([128, H, 2, 258], F32R)
        P.copy(Cr[:], C32[:])
        u = [P.sb([128, 2048]) for _ in range(4)]
        accs = [P.sb([128, 2048]) for _ in range(2)]
        vo = [P.sb([128, 2048]) for _ in range(2)]
        vaug = [P.sb([128, 258], F32R) for _ in range(2)]
        one2 = P.sb([128, 2])
        P.memset(one2[:], 1.0)
        for vb in vaug:
            P.copy(vb[:, 256:258], one2[:])
        qT = [P.sb([128, 2, 128], F32R) for _ in range(2)]
        kT = [P.sb([128, 2, 128], F32R) for _ in range(2)]
        kp = [P.sb([128, 256], F32R) for _ in range(2)]
        SpT = [P.sb([128, 128], F32R) for _ in range(2)]
        hs = [P.sb([128, 256]) for _ in range(2)]
        junk = P.sb([128, 256])
        sg = [P.sb([128, 256]) for _ in range(2)]
        sml = [P.sb([128, 4]) for _ in range(2)]
        tps = [P.ps() for _ in range(2)]
        sps = P.ps()
        ops_ = [P.ps() for _ in range(2)]
        ups = [P.ps() for _ in range(2)]
        k = 0
        for i in range(NC):
            r0 = i * 128
            for j in range(4):
                lo = r0 - 3 + j
                if lo < 0:
                    P.memset(u[j][0:32, :], 0.0)
                    P.dma(u[j][-lo:128, :], z[0:128 + lo, 3072:5120], q="sp" if j % 2 else "act")
                else:
                    P.dma(u[j][:], z[lo:lo + 128, 3072:5120], q="sp" if j % 2 else "act")
            ac = accs[i % 2]
            P.tt(ac[:], u[3][:], wj[:, 3, :], ALU.mult)
            P.tt(ac[:], ac[:], cb[:], ALU.add, eng="pool")
            for j in range(3):
                P.tt(u[j][:], u[j][:], wj[:, j, :], ALU.mult, eng="pool" if j % 2 else "dve")
                P.tt(ac[:], ac[:], u[j][:], ALU.add, eng="dve" if j % 2 else "pool")
            P.act(ac[:], ac[:], AF.Silu)
            v_o = vo[i % 2]
            P.dma(v_o[:], z[r0:r0 + 128, 5120:7168])
            for h in range(H):
                k += 1
                qt, kt_, kpp, spt, va = qT[k % 2], kT[k % 2], kp[k % 2], SpT[k % 2], vaug[k % 2]
                tp = tps[k % 2]
                for dh in range(2):
                    P.transpose(tp[:, dh * 128:(dh + 1) * 128], ac[:, h * 256 + dh * 128: h * 256 + (dh + 1) * 128], ident[:])
                    P.transpose(tp[:, 256 + dh * 128: 256 + (dh + 1) * 128],
                                ac[:, 1024 + h * 256 + dh * 128: 1024 + h * 256 + (dh + 1) * 128], ident[:])
                P.copy(qt[:], tp[:, 0:256].rearrange("p (a t) -> p a t", a=2), eng="act")
                P.copy(kt_[:], tp[:, 256:512].rearrange("p (a t) -> p a t", a=2), eng="act")
                bc_, fc_, gc_ = cols[:, h, 0, i:i + 1], cols[:, h, 1, i:i + 1], cols[:, h, 2, i:i + 1]
                P.ts(kpp[:], ac[:, 1024 + h * 256: 1024 + (h + 1) * 256], bc_, ALU.mult, eng="pool")
                P.copy(va[:, 0:256], v_o[:, h * 256:(h + 1) * 256], eng="pool")
                for dh in range(2):
                    P.mm(sps[:, 0:128], kt_[:, dh, :], qt[:, dh, :], start=(dh == 0), stop=(dh == 1))
                P.stt(spt[:], sps[:, 0:128], bc_, tri[:], ALU.mult, ALU.mult)
                op = ops_[k % 2]
                P.mm(op[:, 0:258], qt[:, 0, :], Cr[:, h, 0, :], start=True, stop=False)
                P.mm(op[:, 0:258], qt[:, 1, :], Cr[:, h, 1, :], start=False, stop=False)
                P.mm(op[:, 0:258], spt[:], va[:], start=False, stop=True)
                S = sml[k % 2]
                P.act(S[:, 0:1], op[:, 256:257], AF.Abs)
                P.ts(S[:, 0:1], S[:, 0:1], fc_, ALU.max)
                P.recip(S[:, 1:2], S[:, 0:1])
                hh = hs[k % 2]
                P.ts(hh[:], op[:, 0:256], S[:, 1:2], ALU.mult)
                P.act(junk[:], hh[:], AF.Square, accum=S[:, 2:3])
                P.act(S[:, 3:4], S[:, 2:3], AF.Sqrt, scale=1.0 / DH, bias=1e-6)
                P.recip(S[:, 3:4], S[:, 3:4])
                s_ = sg[k % 2]
                P.act(s_[:], v_o[:, 1024 + h * 256: 1024 + (h + 1) * 256], AF.Sigmoid)
                P.stt(hh[:], hh[:], S[:, 3:4], hgb[:, h * 256:(h + 1) * 256], ALU.mult, ALU.mult)
                P.tt(hh[:], hh[:], s_[:], ALU.mult, eng="pool")
                P.dma(ymix[r0:r0 + 128, 1024 + h * 256: 1024 + (h + 1) * 256], hh[:])
                if i < NC - 1:
                    for dh in range(2):
                        up = ups[dh]
                        P.mm(up[:, 0:258], kpp[:, dh * 128:(dh + 1) * 128], va[:])
                        P.ts(C32[:, h, dh, :], C32[:, h, dh, :], gc_, ALU.mult)
                        P.stt(C32[:, h, dh, :], up[:, 0:258], gc_, C32[:, h, dh, :], ALU.mult, ALU.add)
                        P.copy(Cr[:, h, dh, :], C32[:, h, dh, :], eng="act")


def stage_moe(P, C, x, w_grp, b_grp, w_exp_r, b_exp_r, w_gate, w_up, w_down, ffn, T=SEQ, n_exp=32):
    ident = C["ident"]
    GT = dram_scratch(P, "moe_GT_%d" % P.nbuf, [32, T])
    with P.scope():
        wr = P.sb([128, 16, 36])
        P.dma(wr[:, :, 0:4], w_grp[:].rearrange("(kt p) n -> p kt n", p=128))
        P.dma(wr[:, :, 4:36], w_exp_r[:].rearrange("(kt p) n -> p kt n", p=128), q="act")
        br = P.sb([128, 36])
        P.dma(br[:, 0:4], b_grp[:].partition_broadcast(128))
        P.dma(br[:, 4:36], b_exp_r[:].partition_broadcast(128), q="act")
        xin = [P.sb([128, D]) for _ in range(2)]
        xT = [P.sb([128, 16, 128]) for _ in range(2)]
        tps = [P.ps() for _ in range(2)]
        lps = P.ps()
        gps = P.ps()
        GTs = P.sb([32, T])
        for i in range(T // 128):
            xi, xt = xin[i % 2], xT[i % 2]
            P.dma(xi[:], x[i * 128:(i + 1) * 128, :], q="sp" if i % 2 else "act")
            for g in range(4):
                tp = tps[g % 2]
                for j in range(4):
                    kt = g * 4 + j
                    P.transpose(tp[:, j * 128:(j + 1) * 128], xi[:, kt * 128:(kt + 1) * 128], ident[:])
                P.copy(xt[:, g * 4:(g + 1) * 4, :], tp[:].rearrange("p (a t) -> p a t", a=4), eng="act" if g % 2 else "dve")
            for kt in range(16):
                P.mm(lps[:, 0:36], xt[:, kt, :], wr[:, kt, :], start=(kt == 0), stop=(kt == 15))
            lg = P.sb([128, 36]); s1 = P.sb([128, 8]); t8 = P.sb([128, 8]); oh = P.sb([128, 4]); junk = P.sb([128, 4])
            lem = P.sb([128, 32]); sel = P.sb([128, 32]); w = P.sb([128, 32]); G = P.sb([128, 32])
            P.tt(lg[:], lps[:, 0:36], br[:], ALU.add)
            P.reduce(s1[:, 0:1], lg[:, 0:4], ALU.max)
            P.ts(s1[:, 1:2], s1[:, 0:1], -1.0, ALU.mult)
            P.act(junk[:], lg[:, 0:4], AF.Exp, bias=s1[:, 1:2], accum=s1[:, 2:3])
            P.recip(s1[:, 3:4], s1[:, 2:3])
            P.ts(oh[:], lg[:, 0:4], s1[:, 0:1], ALU.is_equal)
            P.ts(oh[:], oh[:], 1e30, ALU.mult, -1e30, ALU.add)
            P.tt(lem[:].rearrange("p (g j) -> p g j", j=8), lg[:, 4:36].rearrange("p (g j) -> p g j", j=8),
                 oh[:].bc(2, [128, 4, 8]), ALU.add)
            P.max8(t8[:], lem[:])
            P.ts(sel[:], lem[:], t8[:, 1:2], ALU.is_ge)
            P.ts(s1[:, 4:5], t8[:, 0:1], -1.0, ALU.mult)
            P.act(w[:], lem[:], AF.Exp, bias=s1[:, 4:5])
            P.act(s1[:, 5:6], t8[:, 1:2], AF.Exp, bias=s1[:, 4:5])
            P.ts(s1[:, 5:6], s1[:, 5:6], 1.0, ALU.add)
            P.recip(s1[:, 6:7], s1[:, 5:6])
            P.tt(s1[:, 7:8], s1[:, 6:7], s1[:, 3:4], ALU.mult)
            P.stt(G[:], w[:], s1[:, 7:8], sel[:], ALU.mult, ALU.mult)
            P.transpose(gps[0:32, 0:128], G[:], ident[:])
            P.copy(GTs[:, i * 128:(i + 1) * 128], gps[0:32, 0:128])
        P.dma(GT[:], GTs[:])
    TBm = 512
    with P.scope():
        xt = XT(P, ident, D, tb=TBm)
        acc = P.sb([128, 4, D])
        wg = [P.sb([128, 16, 128], F32R) for _ in range(2)]
        wu = [P.sb([128, 16, 128], F32R) for _ in range(2)]
        wd = [P.sb([128, 4, 512], F32R) for _ in range(2)]
        gtb = [P.sb([128, TBm]) for _ in range(2)]
        hT = [P.sb([128, 4, TBm], F32R) for _ in range(2)]
        sgs = [P.sb([128, TBm]) for _ in range(2)]
        tm = [P.sb([128, TBm]) for _ in range(2)]
        gp = [P.ps() for _ in range(2)]
        up = [P.ps() for _ in range(2)]
        dp = [P.ps() for _ in range(2)]
        kk = 0
        for b0 in range(0, T, TBm):
            xt.load(x, b0, TBm // 128)
            P.memset(acc[:], 0.0)
            for e in range(n_exp):
                gb_ = gtb[e % 2]
                P.dma(gb_[:], GT[e, b0:b0 + TBm].partition_broadcast(128), q="sp")
                h = hT[e % 2]
                for ft in range(4):
                    kk += 1
                    a, b_ = wg[kk % 2], wu[kk % 2]
                    P.dma(a[:], w_gate[e, :, ft * 128:(ft + 1) * 128].rearrange("(kt p) f -> p kt f", p=128), q="pool")
                    P.dma(b_[:], w_up[e, :, ft * 128:(ft + 1) * 128].rearrange("(kt p) f -> p kt f", p=128), q="pool")
                    g_, u_ = gp[kk % 2], up[kk % 2]
                    for kt in range(16):
                        P.mm(g_[:, 0:TBm], a[:, kt, :], xt.xs[:, kt, :], start=(kt == 0), stop=(kt == 15))
                    for kt in range(16):
                        P.mm(u_[:, 0:TBm], b_[:, kt, :], xt.xs[:, kt, :], start=(kt == 0), stop=(kt == 15))
                    sg_, t_ = sgs[kk % 2], tm[kk % 2]
                    P.act(sg_[:], g_[:, 0:TBm], AF.Silu)
                    P.tt(t_[:], sg_[:], u_[:, 0:TBm], ALU.mult)
                    P.tt(h[:, ft, :], t_[:], gb_[:], ALU.mult, eng="pool")
                for dc in range(4):
                    kk += 1
                    wd_ = wd[kk % 2]
                    P.dma(wd_[:], w_down[e, :, dc * 512:(dc + 1) * 512].rearrange("(fc p) d -> p fc d", p=128), q="pool")
                    for tt_i in range(TBm // 128):
                        d_ = dp[(kk * 4 + tt_i) % 2]
                        for fc in range(4):
                            P.mm(d_[:], h[:, fc, tt_i * 128:(tt_i + 1) * 128], wd_[:, fc, :], start=(fc == 0), stop=(fc == 3))
                        P.tt(acc[:, tt_i, dc * 512:(dc + 1) * 512], acc[:, tt_i, dc * 512:(dc + 1) * 512], d_[:], ALU.add)
            P.dma(ffn[b0:b0 + TBm, :].rearrange("(a p) d -> p a d", p=128), acc[:])


def stage_proj(P, C, x, w, out, T, K, N, func=None, bias_b=None):
    with P.scope():
        xt = XT(P, C["ident"], K)
        obs = [P.sb([128, 512]) for _ in range(3)]
        st = {"i": 0}

        def consume(ti, n0, nw, ps):
            k = st["i"]
            st["i"] += 1
            ob = obs[k % 3]
            if func is None:
                P.copy(ob[:, :nw], ps[:, :nw], eng="act" if k % 2 else "dve")
            else:
                P.act(ob[:, :nw], ps[:, :nw], func)
            P.dma(out[ti * 128:(ti + 1) * 128, n0:n0 + nw], ob[:, :nw], q="sp")
        lin_stream(P, xt, x, w, T, N, consume)


def stage_addln(P, x, mix, g, b, y, T=SEQ, is_output=False):
    with P.scope():
        gs = P.sb([128, D])
        bs = P.sb([128, D])
        P.dma(gs[:], g[:].partition_broadcast(128))
        P.dma(bs[:], b[:].partition_broadcast(128), q="act")
        nst = D // 512
        xts = [P.sb([128, D]) for _ in range(2)]
        mts = [P.sb([128, D]) for _ in range(2)]
        ts_ = [P.sb([128, D]) for _ in range(2)]
        sts = [P.sb([128, nst, 6]) for _ in range(2)]
        sm = [P.sb([128, 4]) for _ in range(2)]
        for tt in range(T // 128):
            r = slice(tt * 128, (tt + 1) * 128)
            xt, mt, t, st, S = xts[tt % 2], mts[tt % 2], ts_[tt % 2], sts[tt % 2], sm[tt % 2]
            P.dma(xt[:], x[r, :])
            P.dma(mt[:], mix[r, :], q="act")
            P.stt(t[:], xt[:], DN_ALPHA, mt[:], ALU.mult, ALU.add)
            for j in range(nst):
                P.bn_stats(st[:, j, :], t[:, j * 512:(j + 1) * 512])
            P.bn_aggr(S[:, 0:2], st[:].rearrange("p a s -> p (a s)"))
            P.act(S[:, 2:3], S[:, 1:2], AF.Sqrt, bias=LN_EPS, scale=1.0)
            P.recip(S[:, 3:4], S[:, 2:3])
            P.ts(t[:], t[:], S[:, 0:1], ALU.subtract, S[:, 3:4], ALU.mult)
            P.tt(t[:], t[:], gs[:], ALU.mult, eng="pool")
            P.tt(t[:], t[:], bs[:], ALU.add, eng="pool")
            P.dma(y[r, :], t[:], is_output=is_output)


def even_layer(P, C, W, e, x, xo, S):
    stage_even_inproj(P, C, x, W["ev_w_in"][e], S["z"], S["gT"])
    stage_moba(P, C, S["z"], S["ymix"])
    stage_mlstm(P, C, S["z"], S["gT"], W["ev_gate_bias"][e], W["ev_conv_w"][e], W["ev_conv_b"][e],
                W["ev_hnorm_g"][e], S["ymix"])
    stage_proj(P, C, S["ymix"], W["ev_w_out"][e], S["mix"], SEQ, D, D)


def moe_layer(P, C, W, l, x1, x2, S, is_output=False):
    stage_moe_sparse(P, C, x1, W["moe_w_grp"][l], W["moe_b_grp"][l], W["moe_w_exp_r"][l], W["moe_b_exp_r"][l],
                     W["moe_w_gateT_%d" % l], W["moe_w_upT_%d" % l], W["moe_w_downT_%d" % l], S["ffn"])
    stage_addln(P, x1, S["ffn"], W["ln_ffn_g"][l], W["ln_ffn_b"][l], x2, is_output=is_output)


C_HEADS, C_HD = 32, 64


def stage_rwkv_prep(P, C, W, o, x, S, T=SEQ):
    ident = C["ident"]
    NT = T // 128
    xm = S["xm"]
    with P.scope():
        mub = P.sb([128, 6, D])
        for j in range(6):
            P.dma(mub[:, j, :], W["od_mu"][o, j, :].partition_broadcast(128), q="sp" if j % 2 else "act")
        xs_ = [P.sb([128, D]) for _ in range(2)]
        xp = [P.sb([128, D]) for _ in range(2)]
        om = [P.sb([128, D]) for _ in range(3)]
        k = 0
        for i in range(NT):
            r0 = i * 128
            xt, xq = xs_[i % 2], xp[i % 2]
            P.dma(xt[:], x[r0:r0 + 128, :])
            if i == 0:
                P.memset(xq[0:32, :], 0.0)
                P.dma(xq[1:128, :], x[0:127, :], q="act")
            else:
                P.dma(xq[:], x[r0 - 1:r0 + 127, :], q="act")
            P.tt(xq[:], xq[:], xt[:], ALU.subtract)
            for j in range(6):
                k += 1
                t = om[k % 3]
                e = "pool" if j % 2 else "dve"
                P.tt(t[:], xq[:], mub[:, j, :], ALU.mult, eng=e)
                P.tt(t[:], t[:], xt[:], ALU.add, eng=e)
                P.dma(xm[j, r0:r0 + 128, :], t[:], q="sp" if j % 2 else "act")
    stage_proj(P, C, xm[0], W["od_w_r"][o], S["r"], T, D, D)
    stage_proj(P, C, xm[2], W["od_w_k"][o], S["k"], T, D, D)
    stage_proj(P, C, xm[3], W["od_w_v"][o], S["v"], T, D, D)
    stage_proj(P, C, xm[1], W["od_w1p"][o], S["l1"], T, D, 128, func=AF.Tanh)
    stage_proj(P, C, S["l1"], W["od_w2p"][o], S["wl"], T, 128, D)
    stage_proj(P, C, xm[4], W["od_a1p"][o], S["l2"], T, D, 128)
    stage_proj(P, C, S["l2"], W["od_a2p"][o], S["al"], T, 128, D)
    stage_proj(P, C, xm[5], W["od_g1"][o], S["l3"], T, D, 256, func=AF.Sigmoid)
    stage_proj(P, C, S["l3"], W["od_g2"][o], S["g"], T, 256, D)
    if o > 0:
        stage_proj(P, C, xm[3], W["od_v1p"][o - 1], S["l4"], T, D, 128)
        stage_proj(P, C, S["l4"], W["od_v2p"][o - 1], S["vl"], T, 128, D)
    with P.scope():
        names = ["od_w0", "od_a0", "od_k_k", "od_k_a", "od_r_k"]
        pb = P.sb([128, 6, D])
        for j, n in enumerate(names):
            P.dma(pb[:, j, :], W[n][o].partition_broadcast(128), q="sp" if j % 2 else "act")
        if o > 0:
            P.dma(pb[:, 5, :], W["od_v0"][o - 1].partition_broadcast(128))
        omka = P.sb([128, D])
        P.ts(omka[:], pb[:, 3, :], -1.0, ALU.mult, 1.0, ALU.add)
        B = lambda: [P.sb([128, D]) for _ in range(2)]
        B1 = lambda: [P.sb([128, D])] * 2
        rb, kb, vb, wb, ab, t1b, t2b, vfb = B(), B(), B(), B1(), B1(), B1(), B1(), B1()
        sm = [P.sb([128, 4, 32]) for _ in range(2)]
        vts = [P.sb([128, 16, 128]) for _ in range(2)]
        tps = [P.ps() for _ in range(2)]
        h3 = lambda v: v.rearrange("p (h k) -> p h k", k=64)
        for i in range(NT):
            r = slice(i * 128, (i + 1) * 128)
            q = i % 2
            rt, kt_, vt, wt, at, t1, t2, vf, S_ = rb[q], kb[q], vb[q], wb[q], ab[q], t1b[q], t2b[q], vfb[q], sm[q]
            P.dma(rt[:], S["r"][r, :]); P.dma(kt_[:], S["k"][r, :], q="act")
            P.dma(vt[:], S["v"][r, :]); P.dma(wt[:], S["wl"][r, :], q="act"); P.dma(at[:], S["al"][r, :])
            P.tt(wt[:], wt[:], pb[:, 0, :], ALU.add)
            P.act(wt[:], wt[:], AF.Exp, scale=-1.0)
            P.act(wt[:], wt[:], AF.Ln, bias=1.0)
            P.act(wt[:], wt[:], AF.Exp, scale=-1.0, bias=-0.5)
            if not S.get("_chunked"):
                P.act(wt[:], wt[:], AF.Exp, scale=-1.0)
            P.dma(S["dec"][r, :], wt[:], q="act")
            P.tt(at[:], at[:], pb[:, 1, :], ALU.add, eng="pool")
            P.act(at[:], at[:], AF.Sigmoid)
            if o > 0:
                P.dma(t1[:], S["vl"][r, :]); P.dma(vf[:], S["vfirst"][r, :], q="act")
                P.tt(t1[:], t1[:], pb[:, 5, :], ALU.add, eng="pool")
                P.act(t1[:], t1[:], AF.Sigmoid)
                P.tt(vf[:], vf[:], vt[:], ALU.subtract, eng="pool")
                P.tt(vf[:], vf[:], t1[:], ALU.mult, eng="pool")
                P.tt(vt[:], vt[:], vf[:], ALU.add, eng="pool")
            else:
                P.dma(S["vfirst"][r, :], vt[:], q="act")
            P.dma(S["v2"][r, :], vt[:])
            P.tt(t1[:], kt_[:], pb[:, 2, :], ALU.mult)
            P.tt(t2[:], t1[:], t1[:], ALU.mult)
            P.reduce(S_[:, 0, :], h3(t2[:]), ALU.add)
            P.act(S_[:, 0, :], S_[:, 0, :], AF.Sqrt)
            P.ts(S_[:, 0, :], S_[:, 0, :], 1e-12, ALU.max)
            P.recip(S_[:, 1, :], S_[:, 0, :])
            P.tt(h3(t1[:]), h3(t1[:]), S_[:, 1, :].bc(2, [128, 32, 64]), ALU.mult)
            P.dma(S["kk"][r, :], t1[:])
            P.tt(t2[:], t1[:], at[:], ALU.mult)
            P.dma(S["b"][r, :], t2[:], q="act")
            P.tt(at[:], at[:], pb[:, 3, :], ALU.mult, eng="pool")
            P.tt(at[:], at[:], omka[:], ALU.add, eng="pool")
            P.tt(kt_[:], kt_[:], at[:], ALU.mult)
            P.dma(S["kh"][r, :], kt_[:])
            P.tt(t2[:], rt[:], kt_[:], ALU.mult)
            P.tt(t2[:], t2[:], pb[:, 4, :], ALU.mult)
            P.reduce(S_[:, 2, :], h3(t2[:]), ALU.add)
            P.dma(S["coef"][r, :], S_[:, 2, :], q="act")
            if S.get("_chunked"):
                continue
            vts_ = vts[q]
            for g in range(4):
                tp = tps[g % 2]
                for j in range(4):
                    P.transpose(tp[:, j * 128:(j + 1) * 128], vt[:, (g * 4 + j) * 128:(g * 4 + j + 1) * 128], ident[:])
                P.copy(vts_[:, g * 4:(g + 1) * 4, :], tp[:].rearrange("p (a t) -> p a t", a=4), eng="act" if g % 2 else "dve")
            P.dma(S["vT"][:, r].rearrange("(ft p) t -> p ft t", p=128), vts_[:])


def stage_rwkv_scan(P, C, S, T=SEQ):
    TBK = 64
    with P.scope():
        St = P.sb([64, 32, 64])
        P.memset(St[:], 0.0)
        names = ["dec", "kk", "b", "kh", "r"]
        bufs = {n: [P.sb([64, 32, 64]) for _ in range(2)] for n in names}
        tmp = [P.sb([64, 32, 64]) for _ in range(2)]
        vk = [P.sb([64, 32, 64]) for _ in range(2)]
        u = [P.sb([64, 32]) for _ in range(2)]
        vcol = [P.sb([64, 32, TBK]) for _ in range(2)]
        Yb = [P.sb([64, 32, TBK]) for _ in range(2)]
        for t in range(T):
            tb, tl = t // TBK, t % TBK
            if tl == 0:
                P.dma(vcol[tb % 2][:], S["vT"][:, tb * TBK:(tb + 1) * TBK].rearrange("(h v) t -> v h t", v=64))
            q = t % 2
            cur = {}
            for j, n in enumerate(names):
                bq = bufs[n][q]
                P.dma(bq[:].rearrange("p h k -> p (h k)"), S[n][t, :].partition_broadcast(64),
                      q=("sp", "act", "sp", "act", "sp")[j])
                cur[n] = bq
            vc = vcol[tb % 2][:, :, tl]
            P.tt(vk[q][:], cur["kh"][:], vc.bc(2, [64, 32, 64]), ALU.mult, eng="pool")
            P.tt(tmp[0][:], St[:], cur["kk"][:], ALU.mult)
            P.reduce(u[q][:], tmp[0][:], ALU.add)
            P.tt(St[:], St[:], cur["dec"][:], ALU.mult, eng="pool")
            P.tt(tmp[1][:], cur["b"][:], u[q][:].bc(2, [64, 32, 64]), ALU.mult)
            P.tt(St[:], St[:], tmp[1][:], ALU.subtract)
            P.tt(St[:], St[:], vk[q][:], ALU.add)
            P.tt(tmp[0][:], St[:], cur["r"][:], ALU.mult)
            P.reduce(Yb[tb % 2][:, :, tl], tmp[0][:], ALU.add)
            if tl == TBK - 1:
                P.dma(S["yT"][:, tb * TBK:(tb + 1) * TBK].rearrange("(h v) t -> v h t", v=64), Yb[tb % 2][:])


def stage_rwkv_post(P, C, W, o, S, T=SEQ):
    ident = C["ident"]
    with P.scope():
        pb = P.sb([128, 2, D])
        P.dma(pb[:, 0, :], W["od_lnx_g"][o].partition_broadcast(128))
        P.dma(pb[:, 1, :], W["od_lnx_b"][o].partition_broadcast(128), q="act")
        yts = [P.sb([128, 16, 128]) for _ in range(2)]
        ys = [P.sb([128, D]) for _ in range(2)]
        t2b = [P.sb([128, D]) for _ in range(2)]
        vb = [P.sb([128, D]) for _ in range(2)]
        gb = [P.sb([128, D]) for _ in range(2)]
        sm = [P.sb([128, 5, 32]) for _ in range(2)]
        tps = [P.ps() for _ in range(2)]
        h3 = lambda v: v.rearrange("p (h k) -> p h k", k=64)
        for i in range(T // 128):
            r = slice(i * 128, (i + 1) * 128)
            q = i % 2
            yt, y, t2, vt, gt, S_ = yts[q], ys[q], t2b[q], vb[q], gb[q], sm[q]
            if S.get("_chunked"):
                P.dma(y[:], S["y"][r, :])
            else:
                P.dma(yt[:], S["yT"][:, r].rearrange("(ft p) t -> p ft t", p=128))
            P.dma(vt[:], S["v2"][r, :], q="act")
            P.dma(gt[:], S["g"][r, :])
            P.dma(S_[:, 4, :], S["coef"][r, :], q="act")
            for g in range(0 if S.get("_chunked") else 4):
                tp = tps[g % 2]
                for j in range(4):
                    P.transpose(tp[:, j * 128:(j + 1) * 128], yt[:, g * 4 + j, :], ident[:])
                P.copy(y[:, g * 512:(g + 1) * 512], tp[:], eng="act" if g % 2 else "dve")
            P.reduce(S_[:, 0, :], h3(y[:]), ALU.add)
            P.ts(S_[:, 0, :], S_[:, 0, :], 1.0 / 64, ALU.mult)
            P.tt(h3(y[:]), h3(y[:]), S_[:, 0, :].bc(2, [128, 32, 64]), ALU.subtract)
            P.tt(t2[:], y[:], y[:], ALU.mult, eng="pool")
            P.reduce(S_[:, 1, :], h3(t2[:]), ALU.add)
            P.act(S_[:, 1, :], S_[:, 1, :], AF.Sqrt, scale=1.0 / 64, bias=64e-5)
            P.recip(S_[:, 2, :], S_[:, 1, :])
            P.tt(h3(y[:]), h3(y[:]), S_[:, 2, :].bc(2, [128, 32, 64]), ALU.mult)
            P.tt(y[:], y[:], pb[:, 0, :], ALU.mult, eng="pool")
            P.tt(y[:], y[:], pb[:, 1, :], ALU.add, eng="pool")
            P.tt(h3(vt[:]), h3(vt[:]), S_[:, 4, :].bc(2, [128, 32, 64]), ALU.mult)
            P.tt(y[:], y[:], vt[:], ALU.add)
            P.tt(y[:], y[:], gt[:], ALU.mult, eng="pool")
            P.dma(S["ymix"][r, :], y[:])


def odd_layer(P, C, W, o, x, S):
    stage_rwkv_prep(P, C, W, o, x, S)
    if S.get("_chunked"):
        stage_rwkv_chunkprep(P, C, S)
        stage_rwkv_scan_chunked(P, C, S)
    else:
        stage_rwkv_scan(P, C, S)
    stage_rwkv_post(P, C, W, o, S)
    stage_proj(P, C, S["ymix"], W["od_w_o"][o], S["mix"], SEQ, D, D)


def pad_cols(w, n):
    out = np.zeros(w.shape[:-1] + (n,), np.float32)
    out[..., :w.shape[-1]] = w
    return out


def pad_rows(w, n):
    out = np.zeros(w.shape[:-2] + (n, w.shape[-1]), np.float32)
    out[..., :w.shape[-2], :] = w
    return out


def host_weights(inp):
    W = {k: np.ascontiguousarray(v, dtype=np.float32) for k, v in inp.items() if k != "x"}
    for n in ("moe_w_gate", "moe_w_up"):
        w = W.pop(n)
        wt = np.ascontiguousarray(w.reshape(DEPTH, 32, 16, 128, 512).transpose(0, 1, 3, 2, 4)).reshape(DEPTH, 4096, 8192)
        for l in range(DEPTH):
            W["%sT_%d" % (n, l)] = wt[l]
    w = W.pop("moe_w_down")
    wt = np.ascontiguousarray(w.reshape(DEPTH, 32, 4, 128, 2048).transpose(0, 1, 3, 2, 4)).reshape(DEPTH, 4096, 8192)
    for l in range(DEPTH):
        W["moe_w_downT_%d" % l] = wt[l]
    W["od_w1p"] = pad_cols(W.pop("od_w1"), 128); W["od_w2p"] = pad_rows(W.pop("od_w2"), 128)
    W["od_a1p"] = pad_cols(W.pop("od_a1"), 128); W["od_a2p"] = pad_rows(W.pop("od_a2"), 128)
    W["od_v1p"] = pad_cols(W.pop("od_v1"), 128); W["od_v2p"] = pad_rows(W.pop("od_v2"), 128)
    return W


def make_scratch(P, T=SEQ, chunked=True):
    S = {}
    def mk(n, shape):
        S[n] = dram_scratch(P, "s_" + n, shape)
    mk("z", [T, 7168]); mk("gT", [8, T]); mk("ymix", [T, D]); mk("mix", [T, D]); mk("ffn", [T, D])
    mk("xm", [6, T, D])
    for n in ("r", "k", "v", "v2", "wl", "al", "g", "vl", "vfirst", "dec", "kk", "b", "kh"):
        mk(n, [T, D])
    mk("l1", [T, 128]); mk("l2", [T, 128]); mk("l3", [T, 256]); mk("l4", [T, 128])
    mk("coef", [T, 32]); mk("vT", [D, T]); mk("yT", [D, T])
    mk("xa", [T, D]); mk("xb", [T, D])
    for n in ("kt", "bt", "rt", "kkt", "Kh", "Bh", "y"):
        mk(n, [T, D])
    S["_chunked"] = chunked
    return S


N_ACTIVE = 4


def build_program(wshapes, cshapes):
    from contextlib import ExitStack
    P = Prog()
    x = P.dram_in("x", [SEQ, D])
    W = {k: P.dram_in(k, shp) for k, shp in wshapes.items()}
    Cd = {k: P.dram_in("c_" + k, shp) for k, shp in cshapes.items()}
    out = P.dram_out("out", [SEQ, D])
    with ExitStack() as st:
        P.begin(st)
        C = load_consts(P, Cd)
        S = make_scratch(P)
        cur = x
        for l in range(DEPTH):
            if l % 2 == 0:
                even_layer(P, C, W, l // 2, cur, None, S)
            else:
                odd_layer(P, C, W, l // 2, cur, S)
            stage_addln(P, cur, S["mix"], W["ln_mix_g"][l], W["ln_mix_b"][l], S["xa"])
            last = (l == DEPTH - 1)
            moe_layer(P, C, W, l, S["xa"], out if last else S["xb"], S, is_output=last)
            cur = S["xb"]
        P.barrier()
        nc = P.finish()
    return nc


def kernel(**inputs):
    x = np.ascontiguousarray(np.asarray(inputs["x"], dtype=np.float32))
    Wh = host_weights(inputs)
    cn = make_consts()
    nc = build_program({k: v.shape for k, v in Wh.items()}, {k: v.shape for k, v in cn.items()})
    in_maps = []
    for b in range(N_ACTIVE):
        m = {"x": x[b]}
        m.update(Wh)
        m.update({"c_" + k: v for k, v in cn.items()})
        in_maps.append(m)
    res = run_bass_kernel_spmd(nc, in_maps, core_ids=list(range(N_ACTIVE)))
    return np.stack([res.results[b]["out"] for b in range(N_ACTIVE)], axis=0).astype(np.float32)


def stage_rwkv_chunkprep(P, C, S, T=SEQ):
    with P.scope():
        BT = P.sb([128, 128]); BO = P.sb([128, 128])
        P.dma(BT[:], C["rw_BT"][:]); P.dma(BO[:], C["rw_BO"][:], q="act")
        B2 = lambda: [P.sb([128, D]) for _ in range(2)]
        ewb, kkb, bb, khb, rb = B2(), B2(), B2(), B2(), B2()
        Gn = P.sb([128, D]); GL = P.sb([128, D]); e1 = P.sb([128, D]); o1 = [P.sb([128, D]) for _ in range(2)]
        gps = [P.ps() for _ in range(4)]
        lps = [P.ps() for _ in range(4)]
        k = 0
        for i in range(T // 128):
            r = slice(i * 128, (i + 1) * 128)
            q = i % 2
            ew, kk, b, kh, rr = ewb[q], kkb[q], bb[q], khb[q], rb[q]
            P.dma(ew[:], S["dec"][r, :]); P.dma(kk[:], S["kk"][r, :], q="act"); P.dma(b[:], S["b"][r, :])
            P.dma(kh[:], S["kh"][r, :], q="act"); P.dma(rr[:], S["r"][r, :])
            for c4 in range(4):
                cs = slice(c4 * 512, (c4 + 1) * 512)
                P.mm(gps[c4][:], BT[:], ew[:, cs])
                P.mm(lps[c4][:], BO[:], ew[:, cs])
                P.copy(Gn[:, cs], gps[c4][:], eng="act")
                P.copy(GL[:, cs], lps[c4][:], eng="dve")

            def emit(name, src, ee, eng):
                nonlocal k
                k += 1
                o = o1[k % 2]
                P.tt(o[:], src[:], ee[:], ALU.mult, eng=eng)
                P.dma(S[name][r, :], o[:], q="sp" if k % 2 else "act")
            P.act(e1[:], Gn[:], AF.Exp)
            emit("kt", kh, e1, "dve"); emit("bt", b, e1, "pool")
            P.act(e1[:], Gn[:], AF.Exp, scale=-1.0)
            emit("rt", rr, e1, "dve")
            P.tt(e1[:], ew[:], Gn[:], ALU.subtract, eng="pool")
            P.act(e1[:], e1[:], AF.Exp)
            emit("kkt", kk, e1, "dve")
            P.tt(e1[:], Gn[:], GL[:], ALU.subtract, eng="pool")
            P.act(e1[:], e1[:], AF.Exp)
            emit("Kh", kh, e1, "dve"); emit("Bh", b, e1, "pool")


def stage_rwkv_scan_chunked(P, C, S, T=SEQ):
    ident = C["ident"]
    L, NP = 64, 8
    NCH = T // L
    with P.scope():
        msk = {}
        for n in ("SU", "SL", "UI"):
            msk[n] = P.sb([128, 128])
            P.dma(msk[n][:], C["rw_" + n][:])
        Ibd = ident
        ones2 = P.sb([64, 2]); P.memset(ones2[:], 1.0)
        zt = P.sb([128, NP, 128]); P.memset(zt[:], 0.0)
        BD = lambda: P.sb([128, NP, 128], F32R)
        bT, kT, Pm, PTm, P2m, PT2m, R, RT, Mbr, Mkk, Mkr = (BD() for _ in range(11))
        KR = P.sb([128, NP, 2, 128], F32R)
        Khb = [BD() for _ in range(2)]; Bhb = [BD() for _ in range(2)]
        for t_ in (bT, kT, Khb[0], Khb[1], Bhb[0], Bhb[1]):
            P.copy(t_[:], zt[:])
        P.copy(KR[:, :, 0, :], zt[:]); P.copy(KR[:, :, 1, :], zt[:])
        Vst = [P.sb([128, NP, 64], F32R) for _ in range(2)]
        S32 = P.sb([128, NP, 64]); S0r = P.sb([128, NP, 64], F32R)
        Xsb = P.sb([128, NP, 64], F32R); nU = P.sb([128, NP, 64], F32R); Ysb = P.sb([128, NP, 64])
        tmpS = P.sb([128, NP, 64]); GLc = P.sb([128, NP])
        NCOL = NP * 128
        ld = {n: [P.sb([64, NCOL]) for _ in range(2)] for n in ("kt", "bt", "rt", "kkt", "dec")}
        tpsA = [P.ps() for _ in range(2)]
        gps = [P.ps() for _ in range(2)]
        ips = [P.ps() for _ in range(2)]
        sps = [P.ps() for _ in range(2)]
        g4 = lambda ps: ps[:].rearrange("p (a t) -> p a t", t=128)
        s8 = lambda ps: ps[:].rearrange("p (a t) -> p a t", t=64)
        for half in range(C_HEADS // 2 // NP):
            c0 = half * NCOL
            P.memset(S32[:], 0.0)
            P.copy(S0r[:], S32[:])
            for c in range(NCH):
                t0 = c * L
                q = c % 2
                rows = slice(t0, t0 + L)
                for j, n in enumerate(("kt", "bt", "rt", "kkt", "dec")):
                    P.dma(ld[n][q][:], S[n][rows, c0:c0 + NCOL], q="sp" if j % 2 else "act")
                Kh, Bh, V = Khb[q], Bhb[q], Vst[q]
                for hh in range(2):
                    ps_ = slice(hh * 64, (hh + 1) * 64)
                    src = lambda n: S[n][rows, c0:c0 + NCOL].rearrange("s (p h k) -> s p h k", h=2, k=64)[:, :, hh, :]
                    P.dma(Kh[ps_, :, hh * 64:(hh + 1) * 64], src("Kh"), q="pool")
                    P.dma(Bh[ps_, :, hh * 64:(hh + 1) * 64], src("Bh"), q="pool")
                    P.dma(V[ps_, :, :], src("v2"), q="pool")
                gl = sps[0]
                for p in range(NP):
                    P.mm(gl[:, 2 * p:2 * p + 2], ld["dec"][q][:, p * 128:(p + 1) * 128], ones2[:])
                P.act(GLc[:], gl[:, 0:2 * NP].rearrange("p (a two) -> p a two", two=2)[:, :, 0], AF.Exp, scale=-1.0)
                for n, dst in (("bt", bT[:]), ("kt", kT[:]), ("kkt", KR[:, :, 0, :]), ("rt", KR[:, :, 1, :])):
                    tp = tpsA[0 if n in ("bt", "kkt") else 1]
                    for p in range(NP):
                        P.transpose(tp[:, p * 64:(p + 1) * 64], ld[n][q][:, p * 128:(p + 1) * 128], ident[0:64, 0:64])
                    t3 = s8(tp)
                    P.copy(dst[0:64, :, 0:64], t3[0:64], eng="act")
                    P.copy(dst[64:128, :, 64:128], t3[64:128], eng="dve")
                for p2 in range(NP // 2):
                    pp = slice(2 * p2, 2 * p2 + 2)
                    g1 = gps[0]; g2 = gps[1]
                    for a in range(2):
                        p = 2 * p2 + a
                        rhs = KR[:, p].rearrange("p a t -> p (a t)")
                        P.mm(g1[:, a * 256:(a + 1) * 256], bT[:, p, :], rhs)
                        P.mm(g2[:, a * 256:(a + 1) * 256], kT[:, p, :], rhs)
                    v1 = g1[:].rearrange("p (a b t) -> p a b t", b=2, t=128)
                    v2 = g2[:].rearrange("p (a b t) -> p a b t", b=2, t=128)
                    P.stt(Pm[:, pp, :], v1[:, :, 0, :], -1.0, msk["SU"][:].bc(1, [128, 2, 128]), ALU.mult, ALU.mult)
                    P.tt(Mbr[:, pp, :], v1[:, :, 1, :], msk["UI"][:].bc(1, [128, 2, 128]), ALU.mult)
                    P.tt(Mkk[:, pp, :], v2[:, :, 0, :], msk["SU"][:].bc(1, [128, 2, 128]), ALU.mult)
                    P.tt(Mkr[:, pp, :], v2[:, :, 1, :], msk["UI"][:].bc(1, [128, 2, 128]), ALU.mult)
                for p4 in range(NP // 4):
                    pp = slice(4 * p4, 4 * p4 + 4)
                    i1 = ips[p4 % 2]
                    for a in range(4):
                        p = 4 * p4 + a
                        P.mm(i1[:, a * 128:(a + 1) * 128], KR[:, p, 0, :], bT[:, p, :])
                    P.stt(PTm[:, pp, :], g4(i1), -1.0, msk["SL"][:].bc(1, [128, 4, 128]), ALU.mult, ALU.mult)
                    P.tt(R[:, pp, :], Pm[:, pp, :], Ibd[:].bc(1, [128, 4, 128]), ALU.add, eng="pool")
                    P.tt(RT[:, pp, :], PTm[:, pp, :], Ibd[:].bc(1, [128, 4, 128]), ALU.add, eng="pool")
                A_, AT_, B_, BT_ = Pm, PTm, P2m, PT2m
                for lev in range(5):
                    lastlev = lev == 4
                    for p4 in range(NP // 4):
                        pp = slice(4 * p4, 4 * p4 + 4)
                        i1, i2 = ips[0], ips[1]
                        for a in range(4):
                            p = 4 * p4 + a
                            P.mm(i1[:, a * 128:(a + 1) * 128], AT_[:, p, :], A_[:, p, :])
                            if not lastlev:
                                P.mm(i2[:, a * 128:(a + 1) * 128], A_[:, p, :], AT_[:, p, :])
                        P.copy(B_[:, pp, :], g4(i1), eng="act")
                        if not lastlev:
                            P.copy(BT_[:, pp, :], g4(i2), eng="act")
                        for a in range(4):
                            p = 4 * p4 + a
                            P.mm(i1[:, a * 128:(a + 1) * 128], RT[:, p, :], B_[:, p, :])
                            if not lastlev:
                                P.mm(i2[:, a * 128:(a + 1) * 128], B_[:, p, :], RT[:, p, :])
                        P.tt(R[:, pp, :], R[:, pp, :], g4(i1), ALU.add)
                        if not lastlev:
                            P.tt(RT[:, pp, :], RT[:, pp, :], g4(i2), ALU.add)
                    A_, AT_, B_, BT_ = B_, BT_, A_, AT_
                xs_ = sps[1]
                for p in range(NP):
                    o = xs_[:, p * 64:(p + 1) * 64]
                    P.mm(o, KR[:, p, 0, :], S0r[:, p, :], start=True, stop=False)
                    P.mm(o, Mkk[:, p, :], V[:, p, :], start=False, stop=True)
                P.copy(Xsb[:], s8(xs_), eng="act")
                us_ = sps[0]
                for p in range(NP):
                    P.mm(us_[:, p * 64:(p + 1) * 64], R[:, p, :], Xsb[:, p, :])
                P.act(nU[:], s8(us_), AF.Copy, scale=-1.0)
                ys_ = sps[1]
                for p in range(NP):
                    o = ys_[:, p * 64:(p + 1) * 64]
                    P.mm(o, KR[:, p, 1, :], S0r[:, p, :], start=True, stop=False)
                    P.mm(o, Mbr[:, p, :], nU[:, p, :], start=False, stop=False)
                    P.mm(o, Mkr[:, p, :], V[:, p, :], start=False, stop=True)
                P.copy(Ysb[:], s8(ys_), eng="dve")
                for hh in range(2):
                    dst = S["y"][rows, c0:c0 + NCOL].rearrange("s (p h k) -> s p h k", h=2, k=64)[:, :, hh, :]
                    P.dma(dst, Ysb[hh * 64:(hh + 1) * 64, :, :], q="sp" if hh else "act")
                if c < NCH - 1:
                    ns_ = sps[0]
                    for p in range(NP):
                        o = ns_[:, p * 64:(p + 1) * 64]
                        P.mm(o, Bh[:, p, :], nU[:, p, :], start=True, stop=False)
                        P.mm(o, Kh[:, p, :], V[:, p, :], start=False, stop=True)
                    P.tt(tmpS[:], S32[:], GLc[:].bc(2, [128, NP, 64]), ALU.mult, eng="pool")
                    P.tt(S32[:], tmpS[:], s8(ns_), ALU.add)
                    P.copy(S0r[:], S32[:], eng="act")


I32 = mybir.dt.int32


def stage_moe_sparse(P, C, x, w_grp, b_grp, w_exp_r, b_exp_r, wgT, wuT, wdT, ffn, T=SEQ):
    ident = C["ident"]
    NT = T // 128
    NB = (2 * T + 32 * 127 + 127) // 128
    NR = NB * 128
    xbuf = dram_scratch(P, "moe_xbuf_%d" % P.nbuf, [NR, D])
    ybuf = dram_scratch(P, "moe_ybuf_%d" % P.nbuf, [NR, D])
    meta = dram_scratch(P, "moe_meta_%d" % P.nbuf, [128, NT, 4])
    widx_d = dram_scratch(P, "moe_widx_%d" % P.nbuf, [128, NB], I32)
    with P.scope():
        wr = P.sb([128, 16, 36])
        P.dma(wr[:, :, 0:4], w_grp[:].rearrange("(kt p) n -> p kt n", p=128))
        P.dma(wr[:, :, 4:36], w_exp_r[:].rearrange("(kt p) n -> p kt n", p=128), q="act")
        br = P.sb([128, 36])
        P.dma(br[:, 0:4], b_grp[:].partition_broadcast(128))
        P.dma(br[:, 4:36], b_exp_r[:].partition_broadcast(128), q="act")
        slt = P.sb([128, 128]); ones = P.sb([128, 128]); iop = P.sb([128, 1]); thr = P.sb([128, NB])
        P.dma(slt[:], C["slt"][:]); P.dma(ones[:], C["ones128"][:], q="act"); P.dma(iop[:], C["iota_p"][:])
        P.dma(thr[:], C["blk_thr"][0:NB].partition_broadcast(128), q="act")
        xin = [P.sb([128, D]) for _ in range(2)]
        xT = [P.sb([128, 16, 128]) for _ in range(2)]
        tps = [P.ps() for _ in range(2)]
        lps = P.ps(); cps = P.ps()
        posA = P.sb([128, NT, 32]); s1A = P.sb([128, NT, 32]); s2A = P.sb([128, NT, 32]); gA = P.sb([128, NT, 2])
        carry = P.sb([128, 32]); P.memset(carry[:], 0.0)
        lg = P.sb([128, 36]); s1 = P.sb([128, 8]); t8 = P.sb([128, 8]); oh = P.sb([128, 4]); junk = P.sb([128, 4])
        lem = P.sb([128, 32]); sel = P.sb([128, 32]); w = P.sb([128, 32]); G = P.sb([128, 32]); t32 = P.sb([128, 32])
        for i in range(NT):
            xi, xt = xin[i % 2], xT[i % 2]
            P.dma(xi[:], x[i * 128:(i + 1) * 128, :], q="sp" if i % 2 else "act")
            for g in range(4):
                tp = tps[g % 2]
                for j in range(4):
                    kt = g * 4 + j
                    P.transpose(tp[:, j * 128:(j + 1) * 128], xi[:, kt * 128:(kt + 1) * 128], ident[:])
                P.copy(xt[:, g * 4:(g + 1) * 4, :], tp[:].rearrange("p (a t) -> p a t", a=4), eng="act" if g % 2 else "dve")
            for kt in range(16):
                P.mm(lps[:, 0:36], xt[:, kt, :], wr[:, kt, :], start=(kt == 0), stop=(kt == 15))
            P.tt(lg[:], lps[:, 0:36], br[:], ALU.add)
            P.reduce(s1[:, 0:1], lg[:, 0:4], ALU.max)
            P.ts(s1[:, 1:2], s1[:, 0:1], -1.0, ALU.mult)
            P.act(junk[:], lg[:, 0:4], AF.Exp, bias=s1[:, 1:2], accum=s1[:, 2:3])
            P.recip(s1[:, 3:4], s1[:, 2:3])
            P.ts(oh[:], lg[:, 0:4], s1[:, 0:1], ALU.is_equal)
            P.ts(oh[:], oh[:], 1e30, ALU.mult, -1e30, ALU.add)
            P.tt(lem[:].rearrange("p (g j) -> p g j", j=8), lg[:, 4:36].rearrange("p (g j) -> p g j", j=8),
                 oh[:].bc(2, [128, 4, 8]), ALU.add)
            P.max8(t8[:], lem[:])
            P.ts(sel[:], lem[:], t8[:, 1:2], ALU.is_ge)
            P.ts(s1A[:, i, :], lem[:], t8[:, 0:1], ALU.is_ge)
            P.tt(s2A[:, i, :], sel[:], s1A[:, i, :], ALU.subtract)
            P.ts(s1[:, 4:5], t8[:, 0:1], -1.0, ALU.mult)
            P.act(w[:], lem[:], AF.Exp, bias=s1[:, 4:5])
            P.act(s1[:, 5:6], t8[:, 1:2], AF.Exp, bias=s1[:, 4:5])
            P.ts(s1[:, 5:6], s1[:, 5:6], 1.0, ALU.add)
            P.recip(s1[:, 6:7], s1[:, 5:6])
            P.tt(s1[:, 7:8], s1[:, 6:7], s1[:, 3:4], ALU.mult)
            P.stt(G[:], w[:], s1[:, 7:8], sel[:], ALU.mult, ALU.mult)
            P.tt(t32[:], G[:], s1A[:, i, :], ALU.mult)
            P.reduce(gA[:, i, 0:1], t32[:], ALU.add)
            P.tt(t32[:], G[:], s2A[:, i, :], ALU.mult)
            P.reduce(gA[:, i, 1:2], t32[:], ALU.add)
            P.mm(cps[:, 0:32], slt[:], sel[:])
            P.tt(posA[:, i, :], cps[:, 0:32], carry[:], ALU.add)
            P.mm(cps[:, 32:64], ones[:], sel[:])
            P.tt(carry[:], carry[:], cps[:, 32:64], ALU.add)
        ci = P.sb([128, 32], I32); padf = P.sb([128, 32]); pend = P.sb([128, 32]); pstart = P.sb([128, 32])
        zero32 = P.sb([128, 32]); P.memset(zero32[:], 0.0); one32 = P.sb([128, 32]); P.memset(one32[:], 1.0)
        P.ts(padf[:], carry[:], 127.0, ALU.add)
        P.copy(ci[:], padf[:])
        P.ts(ci[:], ci[:], 7, ALU.arith_shift_right, 7, ALU.arith_shift_left)
        P.copy(padf[:], ci[:])
        P.scan(pend[:], one32[:], padf[:], 0.0, ALU.mult, ALU.add)
        P.tt(pstart[:], pend[:], padf[:], ALU.subtract)
        dsum = P.sb([128, NT, 32]); d12 = P.sb([128, NT, 2]); d12i = P.sb([128, NT, 2], I32)
        P.tt(dsum[:], posA[:], pstart[:].bc(1, [128, NT, 32]), ALU.add)
        tmpd = P.sb([128, NT, 32])
        P.tt(tmpd[:], dsum[:], s1A[:], ALU.mult)
        P.reduce(d12[:, :, 0], tmpd[:], ALU.add)
        P.tt(tmpd[:], dsum[:], s2A[:], ALU.mult)
        P.reduce(d12[:, :, 1], tmpd[:], ALU.add)
        P.copy(d12i[:], d12[:])
        cmp = P.sb([128, NB, 32]); be = P.sb([128, NB]); wix = P.sb([128, NB], I32)
        P.tt(cmp[:], pend[:].bc(1, [128, NB, 32]), thr[:].bc(2, [128, NB, 32]), ALU.is_le)
        P.reduce(be[:], cmp[:], ALU.add)
        P.ts(be[:], be[:], 31.0, ALU.min)
        same = P.sb([128, NB]); P.memset(same[:], 0.0)
        P.tt(same[:, 1:NB], be[:, 1:NB], be[:, 0:NB - 1], ALU.is_equal)
        P.ts(be[:], be[:], 128.0, ALU.mult, iop[:], ALU.add)
        P.stt(be[:], same[:], 100000.0, be[:], ALU.mult, ALU.add)
        P.copy(wix[:], be[:])
        P.dma(widx_d[:], wix[:])
        mt = P.sb([128, NT, 4])
        P.copy(mt[:, :, 0:2], gA[:])
        P.copy(mt[:, :, 2:4].bitcast(I32), d12i[:])
        P.dma(meta[:], mt[:])
        for i in range(NT):
            xi = xin[i % 2]
            P.dma(xi[:], x[i * 128:(i + 1) * 128, :], q="sp" if i % 2 else "act")
            P.scatter_rows(xbuf[:, :], d12i[:, i, 0:1], xi[:], NR)
            P.scatter_rows(xbuf[:, :], d12i[:, i, 1:2], xi[:], NR)
    with P.scope():
        wix = P.sb([128, NB], I32)
        P.dma(wix[:], widx_d[:])
        Wg = P.sb([128, 16, 512], F32R); Wu = P.sb([128, 16, 512], F32R); Wd = P.sb([128, 4, 2048], F32R)
        xb = [P.sb([128, D]) for _ in range(2)]
        xT = [P.sb([128, 16, 128], F32R) for _ in range(2)]
        hb = [P.sb([128, 512]) for _ in range(2)]; sg = [P.sb([128, 512]) for _ in range(2)]
        hT = [P.sb([128, 4, 128], F32R) for _ in range(2)]
        yb = [P.sb([128, D]) for _ in range(2)]
        tps = [P.ps() for _ in range(2)]
        gp = P.ps(); up = P.ps(); hp = P.ps(); dps = [P.ps() for _ in range(2)]
        for j in range(NB):
            q = j % 2
            ix = wix[:, j:j + 1]
            P.gather_rows(Wg[:].rearrange("p a b -> p (a b)"), wgT[:, :], ix, 4096)
            P.gather_rows(Wu[:].rearrange("p a b -> p (a b)"), wuT[:, :], ix, 4096)
            P.gather_rows(Wd[:].rearrange("p a b -> p (a b)"), wdT[:, :], ix, 4096)
            P.dma(xb[q][:], xbuf[j * 128:(j + 1) * 128, :], q="sp" if q else "act")
            for g in range(4):
                tp = tps[g % 2]
                for jj in range(4):
                    kt = g * 4 + jj
                    P.transpose(tp[:, jj * 128:(jj + 1) * 128], xb[q][:, kt * 128:(kt + 1) * 128], ident[:])
                P.copy(xT[q][:, g * 4:(g + 1) * 4, :], tp[:].rearrange("p (a t) -> p a t", a=4), eng="act" if g % 2 else "dve")
            for kt in range(16):
                P.mm(gp[:], xT[q][:, kt, :], Wg[:, kt, :], start=(kt == 0), stop=(kt == 15))
            for kt in range(16):
                P.mm(up[:], xT[q][:, kt, :], Wu[:, kt, :], start=(kt == 0), stop=(kt == 15))
            P.act(sg[q][:], gp[:], AF.Silu)
            P.tt(hb[q][:], sg[q][:], up[:], ALU.mult)
            for fc in range(4):
                P.transpose(hp[:, fc * 128:(fc + 1) * 128], hb[q][:, fc * 128:(fc + 1) * 128], ident[:])
            P.copy(hT[q][:], hp[:].rearrange("p (a t) -> p a t", a=4), eng="act")
            for dc in range(4):
                dp = dps[dc % 2]
                for fc in range(4):
                    P.mm(dp[:], hT[q][:, fc, :], Wd[:, fc, dc * 512:(dc + 1) * 512], start=(fc == 0), stop=(fc == 3))
                P.copy(yb[q][:, dc * 512:(dc + 1) * 512], dp[:], eng="dve" if dc % 2 else "act")
            P.dma(ybuf[j * 128:(j + 1) * 128, :], yb[q][:], q="sp" if q else "act")
    with P.scope():
        mt = P.sb([128, NT, 4])
        P.dma(mt[:], meta[:])
        y1 = [P.sb([128, D]) for _ in range(2)]; y2 = [P.sb([128, D]) for _ in range(2)]
        for i in range(NT):
            q = i % 2
            P.gather_rows(y1[q][:], ybuf[:, :], mt[:, i, 2:3].bitcast(I32), NR)
            P.gather_rows(y2[q][:], ybuf[:, :], mt[:, i, 3:4].bitcast(I32), NR)
            P.ts(y1[q][:], y1[q][:], mt[:, i, 0:1], ALU.mult)
            P.stt(y1[q][:], y2[q][:], mt[:, i, 1:2], y1[q][:], ALU.mult, ALU.add)
            P.dma(ffn[i * 128:(i + 1) * 128, :], y1[q][:], q="sp" if q else "act")
```

```python
import numpy as np
import concourse.bass as bass
import concourse.mybir as mybir
from concourse.bass_utils import run_bass_kernel_spmd
from concourse.alu_op_type import AluOpType as ALU

F32 = mybir.dt.float32
F32R = mybir.dt.float32r
U32 = mybir.dt.uint32
AF = mybir.ActivationFunctionType
AX = mybir.AxisListType

NCORES = 8
N_DMA_SEMS = 24


class Trk:
    __slots__ = ("lw", "rd", "excl")

    def __init__(self, excl=False):
        self.lw = None
        self.rd = []
        self.excl = excl


class V:
    __slots__ = ("ap", "trks")

    def __init__(self, ap, trks):
        self.ap = ap
        self.trks = trks

    def __getitem__(self, k):
        return V(self.ap[k], self.trks)

    def bitcast(self, dt):
        return V(self.ap.bitcast(dt), self.trks)

    def rearrange(self, s, **kw):
        return V(self.ap.rearrange(s, **kw), self.trks)

    def bcast(self, shape):
        return V(self.ap.to_broadcast(shape), self.trks)

    def partition_broadcast(self, n):
        return V(self.ap.partition_broadcast(n), self.trks)

    def bc(self, axis, shape):
        return V(self.ap.unsqueeze(axis).to_broadcast(list(shape)), self.trks)

    @property
    def r(self):
        return V(self.ap.bitcast(F32R), self.trks)


class Buf:
    def __init__(self, t, excl=False):
        self.t = t
        self.trk = Trk(excl)
        self.parts = {}

    def __getitem__(self, k):
        return V(self.t[k], [self.trk])

    def part(self, key, k):
        if key not in self.parts:
            self.parts[key] = Trk()
        return V(self.t[k], [self.parts[key]])

    def whole(self, k=slice(None)):
        return V(self.t[k], [self.trk] + list(self.parts.values()))


class Prog:
    ENG = ("pe", "dve", "act", "pool", "sp")

    def __init__(self):
        self.nc = bass.Bass("TRN2", target_bir_lowering=False)
        self.ops = {e: [] for e in self.ENG}
        self.cnt = {e: 0 for e in self.ENG}
        self.known = {e: {} for e in self.ENG}
        self.stack = None
        self.dma_i = 0
        self.dma_ip = 0
        self.dma_uses = [0] * N_DMA_SEMS
        self.out_events = []
        self.nbuf = 0

    def begin(self, stack):
        self.stack = stack
        nc = self.nc
        self.sems = {}
        for e in ("pe", "dve", "act", "pool"):
            self.sems[e] = stack.enter_context(nc.semaphore("s_" + e))
        for i in range(N_DMA_SEMS):
            self.sems[("d", i)] = stack.enter_context(nc.semaphore("s_d%d" % i))

    def dram_in(self, name, shape, dt=F32):
        return Buf(self.nc.dram_tensor(name, list(shape), dt, kind="ExternalInput").ap())

    def dram_out(self, name, shape, dt=F32):
        return Buf(self.nc.dram_tensor(name, list(shape), dt, kind="ExternalOutput").ap())

    def sb(self, shape, dt=F32, name=None):
        self.nbuf += 1
        t = self.stack.enter_context(self.nc.sbuf_tensor(name or "sb%d" % self.nbuf, list(shape), dt))
        return Buf(t)

    def ps(self, shape=(128, 512), dt=F32, name=None):
        self.nbuf += 1
        t = self.stack.enter_context(self.nc.psum_tensor(name or "ps%d" % self.nbuf, list(shape), dt))
        return Buf(t, excl=True)

    def barrier(self):
        targets = [(e, self.cnt[e]) for e in ("pe", "dve", "act", "pool") if self.cnt[e] > 0]
        targets += [(("d", i), 16 * u) for i, u in enumerate(self.dma_uses) if u > 0]
        sems = self.sems
        for eng in self.ENG:
            kn = self.known[eng]
            waits = []
            for k, v in targets:
                if k == eng or kn.get(k, 0) >= v:
                    continue
                kn[k] = v
                waits.append((k, v))
            if not waits:
                continue

            def emit(e, waits=waits):
                for k, v in waits:
                    e.wait_ge(sems[k], v)
            self.ops[eng].append(emit)

    def scope(self):
        prog = self

        class _Scope:
            def __enter__(s):
                from contextlib import ExitStack
                s.outer = prog.stack
                s.st = ExitStack()
                s.st.__enter__()
                prog.stack = s.st
                return s

            def __exit__(s, *a):
                prog.barrier()
                prog.stack = s.outer
                return s.st.__exit__(*a)
        return _Scope()

    def _deps(self, eng, reads, writes, pe_accum=False):
        deps = {}

        def add(ev):
            if ev is None:
                return
            k, v = ev
            if deps.get(k, 0) < v:
                deps[k] = v
        for r in reads:
            for t in r.trks:
                add(t.lw)
                if t.excl:
                    for ev in t.rd:
                        if ev[0] != eng:
                            add(ev)
        for w in writes:
            for t in w.trks:
                if not (pe_accum and t.lw is not None and t.lw[0] == "pe"):
                    add(t.lw)
                for ev in t.rd:
                    add(ev)
        kn = self.known[eng]
        out = []
        for k, v in deps.items():
            if kn.get(k, 0) >= v:
                continue
            kn[k] = v
            out.append((k, v))
        return out

    def _mark(self, ev, reads, writes):
        for r in reads:
            for t in r.trks:
                t.rd.append(ev)
        for w in writes:
            for t in w.trks:
                t.lw = ev
                t.rd = []

    def op(self, eng, fn, reads, writes, pe_accum=False):
        waits = self._deps(eng, reads, writes, pe_accum)
        self.cnt[eng] += 1
        n = self.cnt[eng]
        sem = self.sems[eng]
        sems = self.sems

        def emit(e):
            for k, v in waits[1:]:
                e.wait_ge(sems[k], v)
            ins = fn(e)
            if waits:
                ins._wait_ge(sems[waits[0][0]], waits[0][1])
            ins.then_inc(sem, 1)
        self.ops[eng].append(emit)
        self._mark((eng, n), reads, writes)

    def _slot(self, q):
        half = N_DMA_SEMS // 2
        if q == "pool":
            i = half + self.dma_ip % half
            self.dma_ip += 1
        else:
            i = self.dma_i % half
            self.dma_i += 1
        return i

    def dma(self, out, in_, q="sp", is_output=False, **kw):
        i = self._slot(q)
        key = ("d", i)
        prev = self.dma_uses[i]
        waits = self._deps(q, [in_], [out])
        if prev > 0 and self.known[q].get(key, 0) < 16 * prev:
            self.known[q][key] = 16 * prev
            waits.append((key, 16 * prev))
        self.dma_uses[i] = prev + 1
        sem = self.sems[key]
        sems = self.sems
        oap, iap = out.ap, in_.ap

        def emit(e):
            for k, v in waits[1:]:
                e.wait_ge(sems[k], v)
            ins = e.dma_start(out=oap, in_=iap, **kw)
            if waits:
                ins._wait_ge(sems[waits[0][0]], waits[0][1])
            ins.then_inc(sem, 16)
        self.ops[q].append(emit)
        ev = (key, 16 * (prev + 1))
        self._mark(ev, [in_], [out])
        if is_output:
            self.out_events.append(ev)

    def allgather(self, out_buf, in_buf):
        i = self._slot("pool")
        key = ("d", i)
        prev = self.dma_uses[i]
        out, in_ = out_buf.whole(), in_buf.whole()
        waits = self._deps("pool", [in_], [out])
        if prev > 0 and self.known["pool"].get(key, 0) < 16 * prev:
            self.known["pool"][key] = 16 * prev
            waits.append((key, 16 * prev))
        self.dma_uses[i] = prev + 1
        sem = self.sems[key]
        sems = self.sems
        oap, iap = out.ap, in_.ap

        def emit(e):
            for k, v in waits:
                e.wait_ge(sems[k], v)
            e.collective_compute("AllGather", ALU.bypass, replica_groups=[list(range(NCORES))],
                                 ins=[iap], outs=[oap]).then_inc(sem, 16)
        self.ops["pool"].append(emit)
        self._mark((key, 16 * (prev + 1)), [in_], [out])

    def pool_dma_custom(self, fn, reads, writes):
        i = self._slot("pool")
        key = ("d", i)
        prev = self.dma_uses[i]
        waits = self._deps("pool", reads, writes)
        if prev > 0 and self.known["pool"].get(key, 0) < 16 * prev:
            self.known["pool"][key] = 16 * prev
            waits.append((key, 16 * prev))
        self.dma_uses[i] = prev + 1
        sem = self.sems[key]
        sems = self.sems

        def emit(e):
            for k, v in waits:
                e.wait_ge(sems[k], v)
            fn(e).then_inc(sem, 16)
        self.ops["pool"].append(emit)
        self._mark((key, 16 * (prev + 1)), reads, writes)

    def scatter_rows(self, out_dram, idx, in_, nrows):
        o, ix, i = out_dram.ap, idx.ap, in_.ap
        self.pool_dma_custom(lambda e: e.indirect_dma_start(
            out=o, out_offset=bass.IndirectOffsetOnAxis(ap=ix, axis=0), in_=i, in_offset=None,
            bounds_check=self._breg(e, nrows - 1), oob_is_err=False), [in_, idx], [out_dram])

    def _breg(self, e, val):
        if not hasattr(self, "_bregs"):
            self._bregs = {}
        if val not in self._bregs:
            self._bregs[val] = e.to_reg(val)
        return self._bregs[val]

    def gather_rows(self, out, in_dram, idx, nrows):
        o, ix, i = out.ap, idx.ap, in_dram.ap

        def fn(e):
            try:
                return e.indirect_dma_start(out=o, out_offset=None, in_=i,
                                            in_offset=bass.IndirectOffsetOnAxis(ap=ix, axis=0),
                                            bounds_check=self._breg(e, nrows - 1), oob_is_err=False)
            except Exception:
                print("GATHER FAIL out", o, "idx", ix, "in", i, flush=True)
                raise
        self.pool_dma_custom(fn, [in_dram, idx], [out])

    def mm(self, out, lhsT, rhs, start=True, stop=True):
        o, l, r = out.ap, lhsT.ap, rhs.ap
        self.op("pe", lambda e: e.matmul(o, l, r, start=start, stop=stop),
                [lhsT, rhs], [out], pe_accum=not start)

    def transpose(self, out, in_, ident):
        o, i, d = out.ap, in_.ap, ident.ap
        self.op("pe", lambda e: e.transpose(o, i, d), [in_, ident], [out])

    def act(self, out, in_, func, bias=None, scale=None, eng="act", accum=None):
        o, i = out.ap, in_.ap
        reads = [in_]
        kw = {}
        if bias is not None:
            if isinstance(bias, V):
                reads.append(bias)
                kw["bias"] = bias.ap
            else:
                kw["bias"] = bias
        if scale is not None:
            if isinstance(scale, V):
                reads.append(scale)
                kw["scale"] = scale.ap
            else:
                kw["scale"] = scale
        writes = [out]
        if accum is not None:
            kw["accum_out"] = accum.ap
            writes.append(accum)
        self.op(eng, lambda e: e.activation(o, i, func, **kw), reads, writes)

    def tt(self, out, a, b, op, eng="dve"):
        o, x, y = out.ap, a.ap, b.ap
        self.op(eng, lambda e: e.tensor_tensor(o, x, y, op), [a, b], [out])

    def ts(self, out, a, s1, op0, s2=None, op1=None, eng="dve", accum=None):
        o, x = out.ap, a.ap
        reads = [a]
        if isinstance(s1, V):
            reads.append(s1)
            s1 = s1.ap
        if isinstance(s2, V):
            reads.append(s2)
            s2 = s2.ap
        writes = [out]
        kw = {}
        if accum is not None:
            kw["accum_out"] = accum.ap
            writes.append(accum)
        if op1 is None:
            self.op(eng, lambda e: e.tensor_scalar(o, x, s1, None, op0, **kw), reads, writes)
        else:
            self.op(eng, lambda e: e.tensor_scalar(o, x, s1, s2, op0, op1, **kw), reads, writes)

    def stt(self, out, a, s, b, op0, op1, eng="dve"):
        o, x, y = out.ap, a.ap, b.ap
        reads = [a, b]
        if isinstance(s, V):
            reads.append(s)
            s = s.ap
        self.op(eng, lambda e: e.scalar_tensor_tensor(o, x, s, y, op0, op1), reads, [out])

    def copy(self, out, in_, eng="dve"):
        o, i = out.ap, in_.ap
        if eng == "act":
            self.op(eng, lambda e: e.copy(o, i), [in_], [out])
        else:
            self.op(eng, lambda e: e.tensor_copy(o, i), [in_], [out])

    def memset(self, out, val, eng="dve"):
        o = out.ap
        self.op(eng, lambda e: e.memset(o, val), [], [out])

    def reduce(self, out, in_, op, axis=None, eng="dve"):
        o, i = out.ap, in_.ap
        ax = axis or AX.X
        self.op(eng, lambda e: e.tensor_reduce(o, i, ax, op), [in_], [out])

    def recip(self, out, in_):
        o, i = out.ap, in_.ap
        self.op("dve", lambda e: e.reciprocal(o, i), [in_], [out])

    def max8(self, out, in_):
        o, i = out.ap, in_.ap
        self.op("dve", lambda e: e.max(o, i), [in_], [out])

    def bn_stats(self, out, in_):
        o, i = out.ap, in_.ap
        self.op("dve", lambda e: e.bn_stats(o, i), [in_], [out])

    def bn_aggr(self, out, in_):
        o, i = out.ap, in_.ap
        self.op("dve", lambda e: e.bn_aggr(o, i), [in_], [out])

    def scan(self, out, d0, d1, init, op0, op1):
        o, a, b = out.ap, d0.ap, d1.ap
        reads = [d0, d1]
        if isinstance(init, V):
            reads.append(init)
            init = init.ap
        self.op("dve", lambda e: e.tensor_tensor_scan(o, a, b, init, op0, op1), reads, [out])

    def finish(self):
        nc = self.nc
        final = {}
        for k, v in self.out_events:
            if final.get(k, 0) < v:
                final[k] = v
        sems = self.sems
        ops = self.ops

        def tail(e):
            for k, v in final.items():
                e.wait_ge(sems[k], v)
        with nc.Block() as block:
            @block.sync
            def _(e):
                for f in ops["sp"]:
                    f(e)
                tail(e)

            @block.tensor
            def _(e):
                for f in ops["pe"]:
                    f(e)

            @block.vector
            def _(e):
                for f in ops["dve"]:
                    f(e)

            @block.scalar
            def _(e):
                for f in ops["act"]:
                    f(e)

            @block.gpsimd
            def _(e):
                for f in ops["pool"]:
                    f(e)
        return nc


D = 2048
SEQ = 4096
DEPTH = 4
DN_ALPHA = 8 ** 0.25
LN_EPS = 1e-5
A_HEADS, A_HD = 8, 128
B_HEADS, B_HD = 4, 256
EVEN_IN = 7176
NEG = -1e30
TB = 1024


def dram_scratch(P, name, shape, dt=F32):
    return Buf(P.nc.dram_tensor(name, list(shape), dt, kind="Internal").ap())


def make_consts():
    c = {}
    c["ident"] = np.eye(128, dtype=np.float32)
    half = 16
    inv = np.power(np.float32(500000.0), -np.arange(half, dtype=np.float32) / half)
    ang = np.arange(SEQ, dtype=np.float32)[:, None] * inv[None, :]
    c["cosE"] = np.tile(np.cos(ang).astype(np.float32), (1, 4))
    c["sinE"] = np.tile(np.sin(ang).astype(np.float32), (1, 4))
    nt, nb = SEQ // 128, SEQ // 256
    qb = np.arange(nt) // 2
    c["moba_pm"] = np.where(np.arange(nb)[None, :] < qb[:, None], 0.0, NEG).astype(np.float32).reshape(-1)
    c["moba_nown"] = (np.arange(nb)[None, :] != qb[:, None]).astype(np.float32).reshape(-1)
    e = np.zeros((16, 16, 128), np.float32)
    for n in range(16):
        e[n, n, :] = 1.0
    c["moba_E"] = e
    kk = np.arange(128)[:, None]
    qq = np.arange(512)[None, :]
    ss, tt_ = np.arange(128)[:, None], np.arange(128)[None, :]
    c["tri_st"] = (tt_ >= ss).astype(np.float32)
    c["slt"] = (ss < tt_).astype(np.float32)
    c["ones128"] = np.ones((128, 128), np.float32)
    c["iota_p"] = np.arange(128, dtype=np.float32).reshape(128, 1)
    c["blk_thr"] = (np.arange(128, dtype=np.float32) * 128.0)
    blk = (ss // 64) == (tt_ // 64)
    c["rw_BT"] = (blk & (ss <= tt_)).astype(np.float32)
    c["rw_BO"] = blk.astype(np.float32)
    c["rw_SU"] = (blk & (ss < tt_)).astype(np.float32)
    c["rw_SL"] = (blk & (ss > tt_)).astype(np.float32)
    c["rw_UI"] = (blk & (ss <= tt_)).astype(np.float32)
    c["moba_cm"] = np.stack([(128 * r + kk <= qq) for r in range(4)], 1).astype(np.float32)
    return c


def load_consts(P, Cd):
    C = dict(Cd)
    ident = P.sb([128, 128], name="ident_sb")
    P.dma(ident[:], Cd["ident"][:])
    C["ident"] = ident
    return C


class XT:
    def __init__(self, P, ident, K, tb=TB):
        self.P, self.K, self.tb = P, K, tb
        self.KT = K // 128
        self.xs = P.sb([128, self.KT, tb], F32R)
        self.xin = [P.sb([128, K]) for _ in range(2)]
        self.tps = [P.ps() for _ in range(2)]
        self.ident = ident
        self.n = 0

    def load(self, x, r0, ntiles, dt=F32R):
        P = self.P
        for i in range(ntiles):
            xi = self.xin[self.n % 2]
            P.dma(xi[:], x[r0 + i * 128: r0 + (i + 1) * 128, :], q="sp" if self.n % 2 else "act")
            gs_ = min(4, self.KT)
            for g in range(self.KT // gs_):
                tp = self.tps[(self.n * (self.KT // gs_) + g) % 2]
                for j in range(gs_):
                    kt = g * gs_ + j
                    P.transpose(tp[:, j * 128:(j + 1) * 128], xi[:, kt * 128:(kt + 1) * 128], self.ident[:])
                P.copy(self.xs[:, g * gs_:(g + 1) * gs_, i * 128:(i + 1) * 128],
                       tp[:, 0:gs_ * 128].rearrange("p (a t) -> p a t", a=gs_), eng="act" if g % 2 else "dve")
            self.n += 1


def lin_stream(P, xt, x, w, T, N, consume, n_lo=0, n_hi=None, wbufs=None, pss=None):
    n_hi = N if n_hi is None else n_hi
    KT = xt.KT
    wbufs = wbufs or [P.sb([128, KT, 512], F32R) for _ in range(2)]
    pss = pss or [P.ps() for _ in range(4)]
    it = 0
    ci = 0
    for b0 in range(0, T, xt.tb):
        nt = min(xt.tb, T - b0) // 128
        xt.load(x, b0, nt)
        for n0 in range(n_lo, n_hi, 512):
            nw = min(512, n_hi - n0)
            wb = wbufs[ci % 2]
            ci += 1
            P.dma(wb[:, :, :nw], w[:, n0:n0 + nw].rearrange("(kt p) n -> p kt n", p=128), q="pool")
            for i in range(nt):
                ps = pss[it % len(pss)]
                it += 1
                for kt in range(KT):
                    P.mm(ps[:, :nw], xt.xs[:, kt, i * 128:(i + 1) * 128], wb[:, kt, :nw],
                         start=(kt == 0), stop=(kt == KT - 1))
                consume(b0 // 128 + i, n0, nw, ps)


def stage_even_inproj(P, C, x, w_in, z, gT, T=SEQ):
    with P.scope():
        xt = XT(P, C["ident"], D)
        ntile = T // 128
        cs = P.sb([128, ntile, 64])
        sn = P.sb([128, ntile, 64])
        P.dma(cs[:], C["cosE"][0:T, :].rearrange("(n p) c -> p n c", p=128))
        P.dma(sn[:], C["sinE"][0:T, :].rearrange("(n p) c -> p n c", p=128), q="act")
        obs = [P.sb([128, 512]) for _ in range(3)]
        tmp = [P.sb([128, 4, 64]) for _ in range(2)]
        wg = P.sb([128, 16, 8], F32R)
        P.dma(wg[:], w_in[:, 7168:7176].rearrange("(kt p) n -> p kt n", p=128), q="pool")
        gps = P.ps()
        gsb = P.sb([8, 512])
        st = {"i": 0}

        def consume(ti, n0, nw, ps):
            k = st["i"]
            st["i"] += 1
            ob = obs[k % 3]
            P.copy(ob[:, :nw], ps[:, :nw], eng="act" if k % 2 else "dve")
            if n0 < 2048:
                o3 = ob[:].rearrange("p (h d) -> p h d", h=4)
                x1, x2 = o3[:, :, 0:16], o3[:, :, 16:32]
                c3 = cs[:, ti, :].rearrange("p (h d) -> p h d", h=4)
                s3 = sn[:, ti, :].rearrange("p (h d) -> p h d", h=4)
                t4 = tmp[k % 2]
                a, b_, c_, d_ = (t4[:, j, :].rearrange("p (h d) -> p h d", h=4) for j in range(4))
                e = "pool" if k % 2 else "dve"
                P.tt(a, x1, c3, ALU.mult, eng=e)
                P.tt(b_, x2, s3, ALU.mult, eng=e)
                P.tt(c_, x2, c3, ALU.mult, eng=e)
                P.tt(d_, x1, s3, ALU.mult, eng=e)
                P.tt(x1, a, b_, ALU.subtract, eng=e)
                P.tt(x2, c_, d_, ALU.add, eng=e)
            P.dma(z[ti * 128:(ti + 1) * 128, n0:n0 + nw], ob[:, :nw], q="sp")

        orig_load = xt.load

        def load_and_gates(xd, r0, nt, **kw):
            orig_load(xd, r0, nt, **kw)
            for h0 in range(0, nt * 128, 512):
                hw = min(512, nt * 128 - h0)
                for kt in range(16):
                    P.mm(gps[0:8, :hw], wg[:, kt, :], xt.xs[:, kt, h0:h0 + hw], start=(kt == 0), stop=(kt == 15))
                P.copy(gsb[:, :hw], gps[0:8, :hw])
                P.dma(gT[:, r0 + h0:r0 + h0 + hw], gsb[:, :hw])
        xt.load = load_and_gates
        lin_stream(P, xt, x, w_in, T, 7168, consume)


def stage_moba(P, C, z, ymix, T=SEQ, upto=3, heads=A_HEADS):
    NT, NBLK = T // 128, T // 256
    scale = A_HD ** -0.5
    with P.scope():
        ident = C["ident"]
        pm = P.sb([128, 32, 16])
        nown = P.sb([128, 32, 16])
        P.dma(pm[:].rearrange("p a b -> p (a b)"), C["moba_pm"][:].partition_broadcast(128))
        P.dma(nown[:].rearrange("p a b -> p (a b)"), C["moba_nown"][:].partition_broadcast(128), q="act")
        Esb = P.sb([128, 16, 128], F32R)
        ez = P.sb([128, 16, 128])
        P.memset(ez[:], 0.0)
        P.copy(Esb[:], ez[:])
        P.dma(Esb[0:16], C["moba_E"][:], q="pool")
        cm = P.sb([128, 4, 512])
        P.dma(cm[:], C["moba_cm"][:])
        qT32 = P.sb([128, T])
        qTr = P.sb([128, T], F32R)
        kTr = P.sb([128, T], F32R)
        vaug = P.sb([128, NT, 130], F32R)
        ones_c = P.sb([128, NT, 2])
        P.memset(ones_c[:], 1.0)
        P.copy(vaug[:, :, 128:130], ones_c[:])
        ksum = P.sb([128, NT])
        kmT = P.sb([128, NBLK])
        biasT = P.sb([128, T], F32R)
        bz = P.sb([128, T])
        P.memset(bz[:], 0.0)
        P.copy(biasT[:], bz[:])
        ld = [P.sb([128, 3, 128]) for _ in range(2)]
        acc = [P.ps() for _ in range(4)]
        sps = [P.ps() for _ in range(2)]
        gps = P.ps()
        tps = P.ps()
        sm = [dict(gm=P.sb([128, 16]), t8=P.sb([128, 8]), b=P.sb([128, 16]), mx=P.sb([128, 8]), m=P.sb([128, 1]))
              for _ in range(2)]
        for S in sm:
            P.memset(S["gm"][:], NEG)
        pTs = [P.sb([128, 512], F32R) for _ in range(3)]
        yo = [P.sb([128, 128]) for _ in range(2)]
        rc = [P.sb([128, 1]) for _ in range(2)]
        for h in range(heads):
            for i in range(NT):
                l = ld[i % 2]
                r = slice(i * 128, (i + 1) * 128)
                src = z[r, :].rearrange("p (a c) -> p a c", c=1024)[:, 0:3, h * 128:(h + 1) * 128]
                P.dma(l[:], src, q="sp" if i % 2 else "act")
                P.transpose(tps[:, 0:128], l[:, 0, :], ident[:])
                P.transpose(tps[:, 128:256], l[:, 1, :], ident[:])
                P.copy(qT32[:, r], tps[:, 0:128], eng="dve")
                P.copy(qTr[:, r], tps[:, 0:128], eng="act")
                P.copy(kTr[:, r], tps[:, 128:256], eng="act")
                P.reduce(ksum[:, i:i + 1], tps[:, 128:256], ALU.add)
                P.copy(vaug[:, i, 0:128], l[:, 2, :], eng="dve")
            k2 = ksum[:].rearrange("p (n two) -> p n two", two=2)
            P.tt(kmT[:], k2[:, :, 0], k2[:, :, 1], ALU.add)
            P.ts(kmT[:], kmT[:], 1.0 / 256.0, ALU.mult)
            if upto < 2:
                continue
            for i in range(NT):
                S = sm[i % 2]
                r = slice(i * 128, (i + 1) * 128)
                P.mm(gps[:, 0:NBLK], qT32[:, r], kmT[:])
                P.tt(S["gm"][:, 0:NBLK], gps[:, 0:NBLK], pm[:, i, 0:NBLK], ALU.add)
                P.max8(S["t8"][:], S["gm"][:])
                P.ts(S["b"][:], S["gm"][:], S["t8"][:, 2:3], ALU.is_ge)
                P.ts(S["b"][:], S["b"][:], 1e30, ALU.mult, -1e30, ALU.add)
                P.tt(S["b"][:], S["b"][:], pm[:, i, :], ALU.add)
                P.tt(S["b"][:], S["b"][:], nown[:, i, :], ALU.mult)
                nch = (i * 128 + 127) // 512 + 1
                for cch in range(nch):
                    sp = sps[cch % 2]
                    P.mm(sp[:], qTr[:, r], kTr[:, cch * 512:(cch + 1) * 512])
                    P.reduce(S["mx"][:, cch:cch + 1], sp[:], ALU.max)
                P.reduce(S["m"][:], S["mx"][:, 0:nch], ALU.max)
                P.ts(S["b"][:], S["b"][:], 1.0 / scale, ALU.mult, S["m"][:], ALU.subtract)
                P.transpose(gps[0:16, 128:256], S["b"][:], ident[:])
                P.copy(biasT[0:16, r], gps[0:16, 128:256], eng="act")
            if upto < 3:
                continue
            k = 0
            for c in range(T // 512):
                q4 = slice(c * 512, (c + 1) * 512)
                last = 4 * c + 3
                for j in range(last + 1):
                    sp = sps[k % 2]
                    pT = pTs[k % 3]
                    k += 1
                    P.mm(sp[:], kTr[:, j * 128:(j + 1) * 128], qTr[:, q4], start=True, stop=False)
                    P.mm(sp[:], Esb[:, j // 2, :], biasT[:, q4], start=False, stop=True)
                    P.act(pT[:], sp[:], AF.Exp, scale=scale)
                    if j >= 4 * c:
                        P.tt(pT[:], pT[:], cm[:, j - 4 * c, :], ALU.mult, eng="pool" if k % 2 else "dve")
                    for t in range(4):
                        if j > 4 * c + t:
                            continue
                        P.mm(acc[t][:, 0:130], pT[:, t * 128:(t + 1) * 128], vaug[:, j, :],
                             start=(j == 0), stop=(j == 4 * c + t))
                for t in range(4):
                    i = 4 * c + t
                    P.recip(rc[t % 2][:], acc[t][:, 128:129])
                    P.ts(yo[t % 2][:], acc[t][:, 0:128], rc[t % 2][:], ALU.mult)
                    P.dma(ymix[i * 128:(i + 1) * 128, h * 128:(h + 1) * 128], yo[t % 2][:])


def stage_mlstm(P, C, z, gT, gate_bias, conv_w, conv_b, hnorm_g, ymix, T=SEQ):
    NC = T // 128
    H, DH = B_HEADS, B_HD
    ident = C["ident"]
    bcol = dram_scratch(P, "ml_bcol_%d" % P.nbuf, [H, 3, 128, NC])
    with P.scope():
        gb = P.sb([1, 8])
        P.dma(gb[:], gate_bias[:].rearrange("(o a) h -> o (a h)", o=1))
        ones_r = P.sb([1, T])
        P.memset(ones_r[:], 1.0)
        one11 = P.sb([1, 2])
        P.memset(one11[:], 1.0)
        ones128 = P.sb([1, 128])
        P.memset(ones128[:], 1.0)
        cps = P.ps()
        ir = P.sb([1, T]); fr = P.sb([1, T]); cl = P.sb([1, T]); a = P.sb([1, T]); A = P.sb([1, T])
        beta = P.sb([1, T]); flo = P.sb([1, T])
        Ap = P.sb([1, NC]); gam = P.sb([1, NC])
        cols = P.sb([128, 3, NC])
        for h in range(H):
            P.dma(ir[:], gT[h:h + 1, :])
            P.dma(fr[:], gT[4 + h:5 + h, :], q="act")
            P.ts(ir[:], ir[:], gb[:, h:h + 1], ALU.add)
            P.ts(fr[:], fr[:], gb[:, 4 + h:5 + h], ALU.add)
            P.act(fr[:], fr[:], AF.Exp, scale=-1.0)
            P.act(fr[:], fr[:], AF.Ln, bias=1.0)
            P.scan(cl[:], ones_r[:], fr[:], 0.0, ALU.mult, ALU.add)
            P.tt(a[:], ir[:], cl[:], ALU.add)
            P.scan(A[:], a[:], a[:], 0.0, ALU.max, ALU.max)
            A3 = A[:].rearrange("o (c l) -> o c l", l=128)
            P.memset(Ap[:], 0.0)
            if NC > 1:
                P.copy(Ap[:, 1:NC], A3[:, 0:NC - 1, 127])
            P.tt(gam[:], Ap[:], A3[:, :, 127], ALU.subtract)
            P.act(gam[:], gam[:], AF.Exp)
            Apb = Ap[:].bc(2, [1, NC, 128])
            P.tt(beta[:].rearrange("o (c l) -> o c l", l=128), a[:].rearrange("o (c l) -> o c l", l=128), Apb, ALU.subtract)
            P.act(beta[:], beta[:], AF.Exp)
            P.ts(beta[:], beta[:], DH ** -0.5, ALU.mult)
            P.tt(flo[:].rearrange("o (c l) -> o c l", l=128), cl[:].rearrange("o (c l) -> o c l", l=128), Apb, ALU.subtract)
            P.act(flo[:], flo[:], AF.Exp)
            for qi, row in enumerate((beta, flo)):
                for c in range(NC):
                    P.mm(cps[:, c:c + 1], row[:, c * 128:(c + 1) * 128], one11[:, 0:1])
                P.copy(cols[:, qi, :], cps[:, 0:NC])
            P.mm(cps[:, 0:NC], ones128[:], gam[:])
            P.copy(cols[:, 2, :], cps[:, 0:NC])
            P.dma(bcol[h].rearrange("q p c -> p q c"), cols[:])
    with P.scope():
        wj = P.sb([128, 4, 2048])
        for j in range(4):
            P.dma(wj[:, j, :], conv_w[j, :].partition_broadcast(128), q="sp" if j % 2 else "act")
        cb = P.sb([128, 2048])
        P.dma(cb[:], conv_b[:].partition_broadcast(128))
        hgb = P.sb([128, 1024])
        P.dma(hgb[:], hnorm_g[:].partition_broadcast(128), q="act")
        cols = P.sb([128, H, 3, NC])
        P.dma(cols[:], bcol[:].rearrange("h q p c -> p h q c"))
        tri = P.sb([128, 128])
        P.dma(tri[:], C["tri_st"][:])
        C32 = P.sb([128, H, 2, 258])
        P.memset(C32[:], 0.0)
        Cr = P.sb([128, H, 2, 258], F32R)
        P.copy(Cr[:], C32[:])
        u = [P.sb([128, 2048]) for _ in range(4)]
        accs = [P.sb([128, 2048]) for _ in range(2)]
        vo = [P.sb([128, 2048]) for _ in range(2)]
        vaug = [P.sb([128, 258], F32R) for _ in range(2)]
        one2 = P.sb([128, 2])
        P.memset(one2[:], 1.0)
        for vb in vaug:
            P.copy(vb[:, 256:258], one2[:])
        qT = [P.sb([128, 2, 128], F32R) for _ in range(2)]
        kT = [P.sb([128, 2, 128], F32R) for _ in range(2)]
        kp = [P.sb([128, 256], F32R) for _ in range(2)]
        SpT = [P.sb([128, 128], F32R) for _ in range(2)]
        hs = [P.sb([128, 256]) for _ in range(2)]
        junk = P.sb([128, 256])
        sg = [P.sb([128, 256]) for _ in range(2)]
        sml = [P.sb([128, 4]) for _ in range(2)]
        tps = [P.ps() for _ in range(2)]
        sps = P.ps()
        ops_ = [P.ps() for _ in range(2)]
        ups = [P.ps() for _ in range(2)]
        k = 0
        for i in range(NC):
            r0 = i * 128
            for j in range(4):
                lo = r0 - 3 + j
                if lo < 0:
                    P.memset(u[j][0:32, :], 0.0)
                    P.dma(u[j][-lo:128, :], z[0:128 + lo, 3072:5120], q="sp" if j % 2 else "act")
                else:
                    P.dma(u[j][:], z[lo:lo + 128, 3072:5120], q="sp" if j % 2 else "act")
            ac = accs[i % 2]
            P.tt(ac[:], u[3][:], wj[:, 3, :], ALU.mult)
            P.tt(ac[:], ac[:], cb[:], ALU.add, eng="pool")
            for j in range(3):
                P.tt(u[j][:], u[j][:], wj[:, j, :], ALU.mult, eng="pool" if j % 2 else "dve")
                P.tt(ac[:], ac[:], u[j][:], ALU.add, eng="dve" if j % 2 else "pool")
            P.act(ac[:], ac[:], AF.Silu)
            v_o = vo[i % 2]
            P.dma(v_o[:], z[r0:r0 + 128, 5120:7168])
            for h in range(H):
                k += 1
                qt, kt_, kpp, spt, va = qT[k % 2], kT[k % 2], kp[k % 2], SpT[k % 2], vaug[k % 2]
                tp = tps[k % 2]
                for dh in range(2):
                    P.transpose(tp[:, dh * 128:(dh + 1) * 128], ac[:, h * 256 + dh * 128: h * 256 + (dh + 1) * 128], ident[:])
                    P.transpose(tp[:, 256 + dh * 128: 256 + (dh + 1) * 128],
                                ac[:, 1024 + h * 256 + dh * 128: 1024 + h * 256 + (dh + 1) * 128], ident[:])
                P.copy(qt[:], tp[:, 0:256].rearrange("p (a t) -> p a t", a=2), eng="act")
                P.copy(kt_[:], tp[:, 256:512].rearrange("p (a t) -> p a t", a=2), eng="act")
                bc_, fc_, gc_ = cols[:, h, 0, i:i + 1], cols[:, h, 1, i:i + 1], cols[:, h, 2, i:i + 1]
                P.ts(kpp[:], ac[:, 1024 + h * 256: 1024 + (h + 1) * 256], bc_, ALU.mult, eng="pool")
                P.copy(va[:, 0:256], v_o[:, h * 256:(h + 1) * 256], eng="pool")
                for dh in range(2):
                    P.mm(sps[:, 0:128], kt_[:, dh, :], qt[:, dh, :], start=(dh == 0), stop=(dh == 1))
                P.stt(spt[:], sps[:, 0:128], bc_, tri[:], ALU.mult, ALU.mult)
                op = ops_[k % 2]
                P.mm(op[:, 0:258], qt[:, 0, :], Cr[:, h, 0, :], start=True, stop=False)
                P.mm(op[:, 0:258], qt[:, 1, :], Cr[:, h, 1, :], start=False, stop=False)
                P.mm(op[:, 0:258], spt[:], va[:], start=False, stop=True)
                S = sml[k % 2]
                P.act(S[:, 0:1], op[:, 256:257], AF.Abs)
                P.ts(S[:, 0:1], S[:, 0:1], fc_, ALU.max)
                P.recip(S[:, 1:2], S[:, 0:1])
                hh = hs[k % 2]
                P.ts(hh[:], op[:, 0:256], S[:, 1:2], ALU.mult)
                P.act(junk[:], hh[:], AF.Square, accum=S[:, 2:3])
                P.act(S[:, 3:4], S[:, 2:3], AF.Sqrt, scale=1.0 / DH, bias=1e-6)
                P.recip(S[:, 3:4], S[:, 3:4])
                s_ = sg[k % 2]
                P.act(s_[:], v_o[:, 1024 + h * 256: 1024 + (h + 1) * 256], AF.Sigmoid)
                P.stt(hh[:], hh[:], S[:, 3:4], hgb[:, h * 256:(h + 1) * 256], ALU.mult, ALU.mult)
                P.tt(hh[:], hh[:], s_[:], ALU.mult, eng="pool")
                P.dma(ymix[r0:r0 + 128, 1024 + h * 256: 1024 + (h + 1) * 256], hh[:])
                if i < NC - 1:
                    for dh in range(2):
                        up = ups[dh]
                        P.mm(up[:, 0:258], kpp[:, dh * 128:(dh + 1) * 128], va[:])
                        P.ts(C32[:, h, dh, :], C32[:, h, dh, :], gc_, ALU.mult)
                        P.stt(C32[:, h, dh, :], up[:, 0:258], gc_, C32[:, h, dh, :], ALU.mult, ALU.add)
                        P.copy(Cr[:, h, dh, :], C32[:, h, dh, :], eng="act")


def stage_moe(P, C, x, w_grp, b_grp, w_exp_r, b_exp_r, w_gate, w_up, w_down, ffn, T=SEQ, n_exp=32):
    ident = C["ident"]
    GT = dram_scratch(P, "moe_GT_%d" % P.nbuf, [32, T])
    with P.scope():
        wr = P.sb([128, 16, 36])
        P.dma(wr[:, :, 0:4], w_grp[:].rearrange("(kt p) n -> p kt n", p=128))
        P.dma(wr[:, :, 4:36], w_exp_r[:].rearrange("(kt p) n -> p kt n", p=128), q="act")
        br = P.sb([128, 36])
        P.dma(br[:, 0:4], b_grp[:].partition_broadcast(128))
        P.dma(br[:, 4:36], b_exp_r[:].partition_broadcast(128), q="act")
        xin = [P.sb([128, D]) for _ in range(2)]
        xT = [P.sb([128, 16, 128]) for _ in range(2)]
        tps = [P.ps() for _ in range(2)]
        lps = P.ps()
        gps = P.ps()
        GTs = P.sb([32, T])
        for i in range(T // 128):
            xi, xt = xin[i % 2], xT[i % 2]
            P.dma(xi[:], x[i * 128:(i + 1) * 128, :], q="sp" if i % 2 else "act")
            for g in range(4):
                tp = tps[g % 2]
                for j in range(4):
                    kt = g * 4 + j
                    P.transpose(tp[:, j * 128:(j + 1) * 128], xi[:, kt * 128:(kt + 1) * 128], ident[:])
                P.copy(xt[:, g * 4:(g + 1) * 4, :], tp[:].rearrange("p (a t) -> p a t", a=4), eng="act" if g % 2 else "dve")
            for kt in range(16):
                P.mm(lps[:, 0:36], xt[:, kt, :], wr[:, kt, :], start=(kt == 0), stop=(kt == 15))
            lg = P.sb([128, 36]); s1 = P.sb([128, 8]); t8 = P.sb([128, 8]); oh = P.sb([128, 4]); junk = P.sb([128, 4])
            lem = P.sb([128, 32]); sel = P.sb([128, 32]); w = P.sb([128, 32]); G = P.sb([128, 32])
            P.tt(lg[:], lps[:, 0:36], br[:], ALU.add)
            P.reduce(s1[:, 0:1], lg[:, 0:4], ALU.max)
            P.ts(s1[:, 1:2], s1[:, 0:1], -1.0, ALU.mult)
            P.act(junk[:], lg[:, 0:4], AF.Exp, bias=s1[:, 1:2], accum=s1[:, 2:3])
            P.recip(s1[:, 3:4], s1[:, 2:3])
            P.ts(oh[:], lg[:, 0:4], s1[:, 0:1], ALU.is_equal)
            P.ts(oh[:], oh[:], 1e30, ALU.mult, -1e30, ALU.add)
            P.tt(lem[:].rearrange("p (g j) -> p g j", j=8), lg[:, 4:36].rearrange("p (g j) -> p g j", j=8),
                 oh[:].bc(2, [128, 4, 8]), ALU.add)
            P.max8(t8[:], lem[:])
            P.ts(sel[:], lem[:], t8[:, 1:2], ALU.is_ge)
            P.ts(s1[:, 4:5], t8[:, 0:1], -1.0, ALU.mult)
            P.act(w[:], lem[:], AF.Exp, bias=s1[:, 4:5])
            P.act(s1[:, 5:6], t8[:, 1:2], AF.Exp, bias=s1[:, 4:5])
            P.ts(s1[:, 5:6], s1[:, 5:6], 1.0, ALU.add)
            P.recip(s1[:, 6:7], s1[:, 5:6])
            P.tt(s1[:, 7:8], s1[:, 6:7], s1[:, 3:4], ALU.mult)
            P.stt(G[:], w[:], s1[:, 7:8], sel[:], ALU.mult, ALU.mult)
            P.transpose(gps[0:32, 0:128], G[:], ident[:])
            P.copy(GTs[:, i * 128:(i + 1) * 128], gps[0:32, 0:128])
        P.dma(GT[:], GTs[:])
    TBm = 512
    with P.scope():
        xt = XT(P, ident, D, tb=TBm)
        acc = P.sb([128, 4, D])
        wg = [P.sb([128, 16, 128], F32R) for _ in range(2)]
        wu = [P.sb([128, 16, 128], F32R) for _ in range(2)]
        wd = [P.sb([128, 4, 512], F32R) for _ in range(2)]
        gtb = [P.sb([128, TBm]) for _ in range(2)]
        hT = [P.sb([128, 4, TBm], F32R) for _ in range(2)]
        sgs = [P.sb([128, TBm]) for _ in range(2)]
        tm = [P.sb([128, TBm]) for _ in range(2)]
        gp = [P.ps() for _ in range(2)]
        up = [P.ps() for _ in range(2)]
        dp = [P.ps() for _ in range(2)]
        kk = 0
        for b0 in range(0, T, TBm):
            xt.load(x, b0, TBm // 128)
            P.memset(acc[:], 0.0)
            for e in range(n_exp):
                gb_ = gtb[e % 2]
                P.dma(gb_[:], GT[e, b0:b0 + TBm].partition_broadcast(128), q="sp")
                h = hT[e % 2]
                for ft in range(4):
                    kk += 1
                    a, b_ = wg[kk % 2], wu[kk % 2]
                    P.dma(a[:], w_gate[e, :, ft * 128:(ft + 1) * 128].rearrange("(kt p) f -> p kt f", p=128), q="pool")
                    P.dma(b_[:], w_up[e, :, ft * 128:(ft + 1) * 128].rearrange("(kt p) f -> p kt f", p=128), q="pool")
                    g_, u_ = gp[kk % 2], up[kk % 2]
                    for kt in range(16):
                        P.mm(g_[:, 0:TBm], a[:, kt, :], xt.xs[:, kt, :], start=(kt == 0), stop=(kt == 15))
                    for kt in range(16):
                        P.mm(u_[:, 0:TBm], b_[:, kt, :], xt.xs[:, kt, :], start=(kt == 0), stop=(kt == 15))
                    sg_, t_ = sgs[kk % 2], tm[kk % 2]
                    P.act(sg_[:], g_[:, 0:TBm], AF.Silu)
                    P.tt(t_[:], sg_[:], u_[:, 0:TBm], ALU.mult)
                    P.tt(h[:, ft, :], t_[:], gb_[:], ALU.mult, eng="pool")
                for dc in range(4):
                    kk += 1
                    wd_ = wd[kk % 2]
                    P.dma(wd_[:], w_down[e, :, dc * 512:(dc + 1) * 512].rearrange("(fc p) d -> p fc d", p=128), q="pool")
                    for tt_i in range(TBm // 128):
                        d_ = dp[(kk * 4 + tt_i) % 2]
                        for fc in range(4):
                            P.mm(d_[:], h[:, fc, tt_i * 128:(tt_i + 1) * 128], wd_[:, fc, :], start=(fc == 0), stop=(fc == 3))
                        P.tt(acc[:, tt_i, dc * 512:(dc + 1) * 512], acc[:, tt_i, dc * 512:(dc + 1) * 512], d_[:], ALU.add)
            P.dma(ffn[b0:b0 + TBm, :].rearrange("(a p) d -> p a d", p=128), acc[:])


def stage_proj(P, C, x, w, out, T, K, N, func=None, bias_b=None):
    with P.scope():
        xt = XT(P, C["ident"], K)
        obs = [P.sb([128, 512]) for _ in range(3)]
        st = {"i": 0}

        def consume(ti, n0, nw, ps):
            k = st["i"]
            st["i"] += 1
            ob = obs[k % 3]
            if func is None:
                P.copy(ob[:, :nw], ps[:, :nw], eng="act" if k % 2 else "dve")
            else:
                P.act(ob[:, :nw], ps[:, :nw], func)
            P.dma(out[ti * 128:(ti + 1) * 128, n0:n0 + nw], ob[:, :nw], q="sp")
        lin_stream(P, xt, x, w, T, N, consume)


def stage_addln(P, x, mix, g, b, y, T=SEQ, is_output=False):
    with P.scope():
        gs = P.sb([128, D])
        bs = P.sb([128, D])
        P.dma(gs[:], g[:].partition_broadcast(128))
        P.dma(bs[:], b[:].partition_broadcast(128), q="act")
        nst = D // 512
        xts = [P.sb([128, D]) for _ in range(2)]
        mts = [P.sb([128, D]) for _ in range(2)]
        ts_ = [P.sb([128, D]) for _ in range(2)]
        sts = [P.sb([128, nst, 6]) for _ in range(2)]
        sm = [P.sb([128, 4]) for _ in range(2)]
        for tt in range(T // 128):
            r = slice(tt * 128, (tt + 1) * 128)
            xt, mt, t, st, S = xts[tt % 2], mts[tt % 2], ts_[tt % 2], sts[tt % 2], sm[tt % 2]
            P.dma(xt[:], x[r, :])
            P.dma(mt[:], mix[r, :], q="act")
            P.stt(t[:], xt[:], DN_ALPHA, mt[:], ALU.mult, ALU.add)
            for j in range(nst):
                P.bn_stats(st[:, j, :], t[:, j * 512:(j + 1) * 512])
            P.bn_aggr(S[:, 0:2], st[:].rearrange("p a s -> p (a s)"))
            P.act(S[:, 2:3], S[:, 1:2], AF.Sqrt, bias=LN_EPS, scale=1.0)
            P.recip(S[:, 3:4], S[:, 2:3])
            P.ts(t[:], t[:], S[:, 0:1], ALU.subtract, S[:, 3:4], ALU.mult)
            P.tt(t[:], t[:], gs[:], ALU.mult, eng="pool")
            P.tt(t[:], t[:], bs[:], ALU.add, eng="pool")
            P.dma(y[r, :], t[:], is_output=is_output)


def even_layer(P, C, W, e, x, xo, S):
    stage_even_inproj(P, C, x, W["ev_w_in"][e], S["z"], S["gT"])
    stage_moba(P, C, S["z"], S["ymix"])
    stage_mlstm(P, C, S["z"], S["gT"], W["ev_gate_bias"][e], W["ev_conv_w"][e], W["ev_conv_b"][e],
                W["ev_hnorm_g"][e], S["ymix"])
    stage_proj(P, C, S["ymix"], W["ev_w_out"][e], S["mix"], SEQ, D, D)


def moe_layer(P, C, W, l, x1, x2, S, is_output=False):
    stage_moe_sparse(P, C, x1, W["moe_w_grp"][l], W["moe_b_grp"][l], W["moe_w_exp_r"][l], W["moe_b_exp_r"][l],
                     W["moe_w_gateT_%d" % l], W["moe_w_upT_%d" % l], W["moe_w_downT_%d" % l], S["ffn"])
    stage_addln(P, x1, S["ffn"], W["ln_ffn_g"][l], W["ln_ffn_b"][l], x2, is_output=is_output)


C_HEADS, C_HD = 32, 64


def stage_rwkv_prep(P, C, W, o, x, S, T=SEQ):
    ident = C["ident"]
    NT = T // 128
    xm = S["xm"]
    with P.scope():
        mub = P.sb([128, 6, D])
        for j in range(6):
            P.dma(mub[:, j, :], W["od_mu"][o, j, :].partition_broadcast(128), q="sp" if j % 2 else "act")
        xs_ = [P.sb([128, D]) for _ in range(2)]
        xp = [P.sb([128, D]) for _ in range(2)]
        om = [P.sb([128, D]) for _ in range(3)]
        k = 0
        for i in range(NT):
            r0 = i * 128
            xt, xq = xs_[i % 2], xp[i % 2]
            P.dma(xt[:], x[r0:r0 + 128, :])
            if i == 0:
                P.memset(xq[0:32, :], 0.0)
                P.dma(xq[1:128, :], x[0:127, :], q="act")
            else:
                P.dma(xq[:], x[r0 - 1:r0 + 127, :], q="act")
            P.tt(xq[:], xq[:], xt[:], ALU.subtract)
            for j in range(6):
                k += 1
                t = om[k % 3]
                e = "pool" if j % 2 else "dve"
                P.tt(t[:], xq[:], mub[:, j, :], ALU.mult, eng=e)
                P.tt(t[:], t[:], xt[:], ALU.add, eng=e)
                P.dma(xm[j, r0:r0 + 128, :], t[:], q="sp" if j % 2 else "act")
    stage_proj(P, C, xm[0], W["od_w_r"][o], S["r"], T, D, D)
    stage_proj(P, C, xm[2], W["od_w_k"][o], S["k"], T, D, D)
    stage_proj(P, C, xm[3], W["od_w_v"][o], S["v"], T, D, D)
    stage_proj(P, C, xm[1], W["od_w1p"][o], S["l1"], T, D, 128, func=AF.Tanh)
    stage_proj(P, C, S["l1"], W["od_w2p"][o], S["wl"], T, 128, D)
    stage_proj(P, C, xm[4], W["od_a1p"][o], S["l2"], T, D, 128)
    stage_proj(P, C, S["l2"], W["od_a2p"][o], S["al"], T, 128, D)
    stage_proj(P, C, xm[5], W["od_g1"][o], S["l3"], T, D, 256, func=AF.Sigmoid)
    stage_proj(P, C, S["l3"], W["od_g2"][o], S["g"], T, 256, D)
    if o > 0:
        stage_proj(P, C, xm[3], W["od_v1p"][o - 1], S["l4"], T, D, 128)
        stage_proj(P, C, S["l4"], W["od_v2p"][o - 1], S["vl"], T, 128, D)
    with P.scope():
        names = ["od_w0", "od_a0", "od_k_k", "od_k_a", "od_r_k"]
        pb = P.sb([128, 6, D])
        for j, n in enumerate(names):
            P.dma(pb[:, j, :], W[n][o].partition_broadcast(128), q="sp" if j % 2 else "act")
        if o > 0:
            P.dma(pb[:, 5, :], W["od_v0"][o - 1].partition_broadcast(128))
        omka = P.sb([128, D])
        P.ts(omka[:], pb[:, 3, :], -1.0, ALU.mult, 1.0, ALU.add)
        B = lambda: [P.sb([128, D]) for _ in range(2)]
        B1 = lambda: [P.sb([128, D])] * 2
        rb, kb, vb, wb, ab, t1b, t2b, vfb = B(), B(), B(), B1(), B1(), B1(), B1(), B1()
        sm = [P.sb([128, 4, 32]) for _ in range(2)]
        vts = [P.sb([128, 16, 128]) for _ in range(2)]
        tps = [P.ps() for _ in range(2)]
        h3 = lambda v: v.rearrange("p (h k) -> p h k", k=64)
        for i in range(NT):
            r = slice(i * 128, (i + 1) * 128)
            q = i % 2
            rt, kt_, vt, wt, at, t1, t2, vf, S_ = rb[q], kb[q], vb[q], wb[q], ab[q], t1b[q], t2b[q], vfb[q], sm[q]
            P.dma(rt[:], S["r"][r, :]); P.dma(kt_[:], S["k"][r, :], q="act")
            P.dma(vt[:], S["v"][r, :]); P.dma(wt[:], S["wl"][r, :], q="act"); P.dma(at[:], S["al"][r, :])
            P.tt(wt[:], wt[:], pb[:, 0, :], ALU.add)
            P.act(wt[:], wt[:], AF.Exp, scale=-1.0)
            P.act(wt[:], wt[:], AF.Ln, bias=1.0)
            P.act(wt[:], wt[:], AF.Exp, scale=-1.0, bias=-0.5)
            if not S.get("_chunked"):
                P.act(wt[:], wt[:], AF.Exp, scale=-1.0)
            P.dma(S["dec"][r, :], wt[:], q="act")
            P.tt(at[:], at[:], pb[:, 1, :], ALU.add, eng="pool")
            P.act(at[:], at[:], AF.Sigmoid)
            if o > 0:
                P.dma(t1[:], S["vl"][r, :]); P.dma(vf[:], S["vfirst"][r, :], q="act")
                P.tt(t1[:], t1[:], pb[:, 5, :], ALU.add, eng="pool")
                P.act(t1[:], t1[:], AF.Sigmoid)
                P.tt(vf[:], vf[:], vt[:], ALU.subtract, eng="pool")
                P.tt(vf[:], vf[:], t1[:], ALU.mult, eng="pool")
                P.tt(vt[:], vt[:], vf[:], ALU.add, eng="pool")
            else:
                P.dma(S["vfirst"][r, :], vt[:], q="act")
            P.dma(S["v2"][r, :], vt[:])
            P.tt(t1[:], kt_[:], pb[:, 2, :], ALU.mult)
            P.tt(t2[:], t1[:], t1[:], ALU.mult)
            P.reduce(S_[:, 0, :], h3(t2[:]), ALU.add)
            P.act(S_[:, 0, :], S_[:, 0, :], AF.Sqrt)
            P.ts(S_[:, 0, :], S_[:, 0, :], 1e-12, ALU.max)
            P.recip(S_[:, 1, :], S_[:, 0, :])
            P.tt(h3(t1[:]), h3(t1[:]), S_[:, 1, :].bc(2, [128, 32, 64]), ALU.mult)
            P.dma(S["kk"][r, :], t1[:])
            P.tt(t2[:], t1[:], at[:], ALU.mult)
            P.dma(S["b"][r, :], t2[:], q="act")
            P.tt(at[:], at[:], pb[:, 3, :], ALU.mult, eng="pool")
            P.tt(at[:], at[:], omka[:], ALU.add, eng="pool")
            P.tt(kt_[:], kt_[:], at[:], ALU.mult)
            P.dma(S["kh"][r, :], kt_[:])
            P.tt(t2[:], rt[:], kt_[:], ALU.mult)
            P.tt(t2[:], t2[:], pb[:, 4, :], ALU.mult)
            P.reduce(S_[:, 2, :], h3(t2[:]), ALU.add)
            P.dma(S["coef"][r, :], S_[:, 2, :], q="act")
            if S.get("_chunked"):
                continue
            vts_ = vts[q]
            for g in range(4):
                tp = tps[g % 2]
                for j in range(4):
                    P.transpose(tp[:, j * 128:(j + 1) * 128], vt[:, (g * 4 + j) * 128:(g * 4 + j + 1) * 128], ident[:])
                P.copy(vts_[:, g * 4:(g + 1) * 4, :], tp[:].rearrange("p (a t) -> p a t", a=4), eng="act" if g % 2 else "dve")
            P.dma(S["vT"][:, r].rearrange("(ft p) t -> p ft t", p=128), vts_[:])


def stage_rwkv_scan(P, C, S, T=SEQ):
    TBK = 64
    with P.scope():
        St = P.sb([64, 32, 64])
        P.memset(St[:], 0.0)
        names = ["dec", "kk", "b", "kh", "r"]
        bufs = {n: [P.sb([64, 32, 64]) for _ in range(2)] for n in names}
        tmp = [P.sb([64, 32, 64]) for _ in range(2)]
        vk = [P.sb([64, 32, 64]) for _ in range(2)]
        u = [P.sb([64, 32]) for _ in range(2)]
        vcol = [P.sb([64, 32, TBK]) for _ in range(2)]
        Yb = [P.sb([64, 32, TBK]) for _ in range(2)]
        for t in range(T):
            tb, tl = t // TBK, t % TBK
            if tl == 0:
                P.dma(vcol[tb % 2][:], S["vT"][:, tb * TBK:(tb + 1) * TBK].rearrange("(h v) t -> v h t", v=64))
            q = t % 2
            cur = {}
            for j, n in enumerate(names):
                bq = bufs[n][q]
                P.dma(bq[:].rearrange("p h k -> p (h k)"), S[n][t, :].partition_broadcast(64),
                      q=("sp", "act", "sp", "act", "sp")[j])
                cur[n] = bq
            vc = vcol[tb % 2][:, :, tl]
            P.tt(vk[q][:], cur["kh"][:], vc.bc(2, [64, 32, 64]), ALU.mult, eng="pool")
            P.tt(tmp[0][:], St[:], cur["kk"][:], ALU.mult)
            P.reduce(u[q][:], tmp[0][:], ALU.add)
            P.tt(St[:], St[:], cur["dec"][:], ALU.mult, eng="pool")
            P.tt(tmp[1][:], cur["b"][:], u[q][:].bc(2, [64, 32, 64]), ALU.mult)
            P.tt(St[:], St[:], tmp[1][:], ALU.subtract)
            P.tt(St[:], St[:], vk[q][:], ALU.add)
            P.tt(tmp[0][:], St[:], cur["r"][:], ALU.mult)
            P.reduce(Yb[tb % 2][:, :, tl], tmp[0][:], ALU.add)
            if tl == TBK - 1:
                P.dma(S["yT"][:, tb * TBK:(tb + 1) * TBK].rearrange("(h v) t -> v h t", v=64), Yb[tb % 2][:])


def stage_rwkv_post(P, C, W, o, S, T=SEQ):
    ident = C["ident"]
    with P.scope():
        pb = P.sb([128, 2, D])
        P.dma(pb[:, 0, :], W["od_lnx_g"][o].partition_broadcast(128))
        P.dma(pb[:, 1, :], W["od_lnx_b"][o].partition_broadcast(128), q="act")
        yts = [P.sb([128, 16, 128]) for _ in range(2)]
        ys = [P.sb([128, D]) for _ in range(2)]
        t2b = [P.sb([128, D]) for _ in range(2)]
        vb = [P.sb([128, D]) for _ in range(2)]
        gb = [P.sb([128, D]) for _ in range(2)]
        sm = [P.sb([128, 5, 32]) for _ in range(2)]
        tps = [P.ps() for _ in range(2)]
        h3 = lambda v: v.rearrange("p (h k) -> p h k", k=64)
        for i in range(T // 128):
            r = slice(i * 128, (i + 1) * 128)
            q = i % 2
            yt, y, t2, vt, gt, S_ = yts[q], ys[q], t2b[q], vb[q], gb[q], sm[q]
            if S.get("_chunked"):
                P.dma(y[:], S["y"][r, :])
            else:
                P.dma(yt[:], S["yT"][:, r].rearrange("(ft p) t -> p ft t", p=128))
            P.dma(vt[:], S["v2"][r, :], q="act")
            P.dma(gt[:], S["g"][r, :])
            P.dma(S_[:, 4, :], S["coef"][r, :], q="act")
            for g in range(0 if S.get("_chunked") else 4):
                tp = tps[g % 2]
                for j in range(4):
                    P.transpose(tp[:, j * 128:(j + 1) * 128], yt[:, g * 4 + j, :], ident[:])
                P.copy(y[:, g * 512:(g + 1) * 512], tp[:], eng="act" if g % 2 else "dve")
            P.reduce(S_[:, 0, :], h3(y[:]), ALU.add)
            P.ts(S_[:, 0, :], S_[:, 0, :], 1.0 / 64, ALU.mult)
            P.tt(h3(y[:]), h3(y[:]), S_[:, 0, :].bc(2, [128, 32, 64]), ALU.subtract)
            P.tt(t2[:], y[:], y[:], ALU.mult, eng="pool")
            P.reduce(S_[:, 1, :], h3(t2[:]), ALU.add)
            P.act(S_[:, 1, :], S_[:, 1, :], AF.Sqrt, scale=1.0 / 64, bias=64e-5)
            P.recip(S_[:, 2, :], S_[:, 1, :])
            P.tt(h3(y[:]), h3(y[:]), S_[:, 2, :].bc(2, [128, 32, 64]), ALU.mult)
            P.tt(y[:], y[:], pb[:, 0, :], ALU.mult, eng="pool")
            P.tt(y[:], y[:], pb[:, 1, :], ALU.add, eng="pool")
            P.tt(h3(vt[:]), h3(vt[:]), S_[:, 4, :].bc(2, [128, 32, 64]), ALU.mult)
            P.tt(y[:], y[:], vt[:], ALU.add)
            P.tt(y[:], y[:], gt[:], ALU.mult, eng="pool")
            P.dma(S["ymix"][r, :], y[:])


def odd_layer(P, C, W, o, x, S):
    stage_rwkv_prep(P, C, W, o, x, S)
    if S.get("_chunked"):
        stage_rwkv_chunkprep(P, C, S)
        stage_rwkv_scan_chunked(P, C, S)
    else:
        stage_rwkv_scan(P, C, S)
    stage_rwkv_post(P, C, W, o, S)
    stage_proj(P, C, S["ymix"], W["od_w_o"][o], S["mix"], SEQ, D, D)


def pad_cols(w, n):
    out = np.zeros(w.shape[:-1] + (n,), np.float32)
    out[..., :w.shape[-1]] = w
    return out


def pad_rows(w, n):
    out = np.zeros(w.shape[:-2] + (n, w.shape[-1]), np.float32)
    out[..., :w.shape[-2], :] = w
    return out


def host_weights(inp):
    W = {k: np.ascontiguousarray(v, dtype=np.float32) for k, v in inp.items() if k != "x"}
    for n in ("moe_w_gate", "moe_w_up"):
        if n not in W:
            continue
        w = W.pop(n)
        wt = np.ascontiguousarray(w.reshape(DEPTH, 32, 16, 128, 512).transpose(0, 1, 3, 2, 4)).reshape(DEPTH, 4096, 8192)
        for l in range(DEPTH):
            W["%sT_%d" % (n, l)] = wt[l]
    if "moe_w_down" in W:
        w = W.pop("moe_w_down")
        wt = np.ascontiguousarray(w.reshape(DEPTH, 32, 4, 128, 2048).transpose(0, 1, 3, 2, 4)).reshape(DEPTH, 4096, 8192)
        for l in range(DEPTH):
            W["moe_w_downT_%d" % l] = wt[l]
    W["od_w1p"] = pad_cols(W.pop("od_w1"), 128); W["od_w2p"] = pad_rows(W.pop("od_w2"), 128)
    W["od_a1p"] = pad_cols(W.pop("od_a1"), 128); W["od_a2p"] = pad_rows(W.pop("od_a2"), 128)
    W["od_v1p"] = pad_cols(W.pop("od_v1"), 128); W["od_v2p"] = pad_rows(W.pop("od_v2"), 128)
    return W


def make_scratch(P, T=SEQ, chunked=True):
    S = {}
    def mk(n, shape):
        S[n] = dram_scratch(P, "s_" + n, shape)
    mk("z", [T, 7168]); mk("gT", [8, T]); mk("ymix", [T, D]); mk("mix", [T, D]); mk("ffn", [T, D])
    mk("xm", [6, T, D])
    for n in ("r", "k", "v", "v2", "wl", "al", "g", "vl", "vfirst", "dec", "kk", "b", "kh"):
        mk(n, [T, D])
    mk("l1", [T, 128]); mk("l2", [T, 128]); mk("l3", [T, 256]); mk("l4", [T, 128])
    mk("coef", [T, 32]); mk("vT", [D, T]); mk("yT", [D, T])
    mk("xa", [T, D]); mk("xb", [T, D])
    for n in ("kt", "bt", "rt", "kkt", "Kh", "Bh", "y"):
        mk(n, [T, D])
    S["_chunked"] = chunked
    return S


N_ACTIVE = 4


def build_program(wshapes, cshapes):
    from contextlib import ExitStack
    P = Prog()
    x = P.dram_in("x", [SEQ, D])
    W = {k: P.dram_in(k, shp) for k, shp in wshapes.items()}
    Cd = {k: P.dram_in("c_" + k, shp) for k, shp in cshapes.items()}
    out = P.dram_out("out", [SEQ, D])
    with ExitStack() as st:
        P.begin(st)
        C = load_consts(P, Cd)
        S = make_scratch(P)
        cur = x
        for l in range(DEPTH):
            if l % 2 == 0:
                even_layer(P, C, W, l // 2, cur, None, S)
            else:
                odd_layer(P, C, W, l // 2, cur, S)
            stage_addln(P, cur, S["mix"], W["ln_mix_g"][l], W["ln_mix_b"][l], S["xa"])
            last = (l == DEPTH - 1)
            moe_layer(P, C, W, l, S["xa"], out if last else S["xb"], S, is_output=last)
            cur = S["xb"]
        P.barrier()
        nc = P.finish()
    return nc


def kernel(**inputs):
    x = np.ascontiguousarray(np.asarray(inputs["x"], dtype=np.float32))
    Wh = host_weights(inputs)
    cn = make_consts()
    nc = build_program({k: v.shape for k, v in Wh.items()}, {k: v.shape for k, v in cn.items()})
    in_maps = []
    for b in range(N_ACTIVE):
        m = {"x": x[b]}
        m.update(Wh)
        m.update({"c_" + k: v for k, v in cn.items()})
        in_maps.append(m)
    res = run_bass_kernel_spmd(nc, in_maps, core_ids=list(range(N_ACTIVE)))
    return np.stack([res.results[b]["out"] for b in range(N_ACTIVE)], axis=0).astype(np.float32)


def stage_rwkv_chunkprep(P, C, S, T=SEQ):
    with P.scope():
        BT = P.sb([128, 128]); BO = P.sb([128, 128])
        P.dma(BT[:], C["rw_BT"][:]); P.dma(BO[:], C["rw_BO"][:], q="act")
        B2 = lambda: [P.sb([128, D]) for _ in range(2)]
        ewb, kkb, bb, khb, rb = B2(), B2(), B2(), B2(), B2()
        Gn = P.sb([128, D]); GL = P.sb([128, D]); e1 = P.sb([128, D]); o1 = [P.sb([128, D]) for _ in range(2)]
        gps = [P.ps() for _ in range(4)]
        lps = [P.ps() for _ in range(4)]
        k = 0
        for i in range(T // 128):
            r = slice(i * 128, (i + 1) * 128)
            q = i % 2
            ew, kk, b, kh, rr = ewb[q], kkb[q], bb[q], khb[q], rb[q]
            P.dma(ew[:], S["dec"][r, :]); P.dma(kk[:], S["kk"][r, :], q="act"); P.dma(b[:], S["b"][r, :])
            P.dma(kh[:], S["kh"][r, :], q="act"); P.dma(rr[:], S["r"][r, :])
            for c4 in range(4):
                cs = slice(c4 * 512, (c4 + 1) * 512)
                P.mm(gps[c4][:], BT[:], ew[:, cs])
                P.mm(lps[c4][:], BO[:], ew[:, cs])
                P.copy(Gn[:, cs], gps[c4][:], eng="act")
                P.copy(GL[:, cs], lps[c4][:], eng="dve")

            def emit(name, src, ee, eng):
                nonlocal k
                k += 1
                o = o1[k % 2]
                P.tt(o[:], src[:], ee[:], ALU.mult, eng=eng)
                P.dma(S[name][r, :], o[:], q="sp" if k % 2 else "act")
            P.act(e1[:], Gn[:], AF.Exp)
            emit("kt", kh, e1, "dve"); emit("bt", b, e1, "pool")
            P.act(e1[:], Gn[:], AF.Exp, scale=-1.0)
            emit("rt", rr, e1, "dve")
            P.tt(e1[:], ew[:], Gn[:], ALU.subtract, eng="pool")
            P.act(e1[:], e1[:], AF.Exp)
            emit("kkt", kk, e1, "dve")
            P.tt(e1[:], Gn[:], GL[:], ALU.subtract, eng="pool")
            P.act(e1[:], e1[:], AF.Exp)
            emit("Kh", kh, e1, "dve"); emit("Bh", b, e1, "pool")


def stage_rwkv_scan_chunked(P, C, S, T=SEQ):
    ident = C["ident"]
    L, NP = 64, 8
    NCH = T // L
    with P.scope():
        msk = {}
        for n in ("SU", "SL", "UI"):
            msk[n] = P.sb([128, 128])
            P.dma(msk[n][:], C["rw_" + n][:])
        Ibd = ident
        ones2 = P.sb([64, 2]); P.memset(ones2[:], 1.0)
        zt = P.sb([128, NP, 128]); P.memset(zt[:], 0.0)
        BD = lambda: P.sb([128, NP, 128], F32R)
        bT, kT, Pm, PTm, R, Mbr, Mkk, Mkr = (BD() for _ in range(8))
        KR = P.sb([128, NP, 2, 128], F32R)
        PTc = [P.sb([128, NP, 2, 128], F32R) for _ in range(2)]
        PTt = [BD() for _ in range(2)]
        Khb = [BD() for _ in range(2)]; Bhb = [BD() for _ in range(2)]
        for t_ in (bT, kT, Khb[0], Khb[1], Bhb[0], Bhb[1]):
            P.copy(t_[:], zt[:])
        P.copy(KR[:, :, 0, :], zt[:]); P.copy(KR[:, :, 1, :], zt[:])
        Vst = [P.sb([128, NP, 64], F32R) for _ in range(2)]
        S32 = P.sb([128, NP, 64]); S0r = P.sb([128, NP, 64], F32R)
        Xsb = P.sb([128, NP, 64], F32R); nU = P.sb([128, NP, 64], F32R); Ysb = P.sb([128, NP, 64])
        tmpS = P.sb([128, NP, 64]); GLc = P.sb([128, NP])
        NCOL = NP * 128
        ld = {n: [P.sb([64, NCOL]) for _ in range(2)] for n in ("kt", "bt", "rt", "kkt", "dec")}
        tpsA = [P.ps() for _ in range(2)]
        gps = [P.ps() for _ in range(2)]
        ips = [P.ps() for _ in range(2)]
        sps = [P.ps() for _ in range(2)]
        g4 = lambda ps: ps[:].rearrange("p (a t) -> p a t", t=128)
        s8 = lambda ps: ps[:].rearrange("p (a t) -> p a t", t=64)
        for half in range(C_HEADS // 2 // NP):
            c0 = half * NCOL
            P.memset(S32[:], 0.0)
            P.copy(S0r[:], S32[:])
            for c in range(NCH):
                t0 = c * L
                q = c % 2
                rows = slice(t0, t0 + L)
                for j, n in enumerate(("kt", "bt", "rt", "kkt", "dec")):
                    P.dma(ld[n][q][:], S[n][rows, c0:c0 + NCOL], q="sp" if j % 2 else "act")
                Kh, Bh, V = Khb[q], Bhb[q], Vst[q]
                for hh in range(2):
                    ps_ = slice(hh * 64, (hh + 1) * 64)
                    src = lambda n: S[n][rows, c0:c0 + NCOL].rearrange("s (p h k) -> s p h k", h=2, k=64)[:, :, hh, :]
                    P.dma(Kh[ps_, :, hh * 64:(hh + 1) * 64], src("Kh"), q="pool")
                    P.dma(Bh[ps_, :, hh * 64:(hh + 1) * 64], src("Bh"), q="pool")
                    P.dma(V[ps_, :, :], src("v2"), q="pool")
                gl = sps[0]
                for p in range(NP):
                    P.mm(gl[:, 2 * p:2 * p + 2], ld["dec"][q][:, p * 128:(p + 1) * 128], ones2[:])
                P.act(GLc[:], gl[:, 0:2 * NP].rearrange("p (a two) -> p a two", two=2)[:, :, 0], AF.Exp, scale=-1.0)
                for n, dst in (("bt", bT[:]), ("kt", kT[:]), ("kkt", KR[:, :, 0, :]), ("rt", KR[:, :, 1, :])):
                    tp = tpsA[0 if n in ("bt", "kkt") else 1]
                    for p in range(NP):
                        P.transpose(tp[:, p * 64:(p + 1) * 64], ld[n][q][:, p * 128:(p + 1) * 128], ident[0:64, 0:64])
                    t3 = s8(tp)
                    P.copy(dst[0:64, :, 0:64], t3[0:64], eng="act")
                    P.copy(dst[64:128, :, 64:128], t3[64:128], eng="dve")
                for p2 in range(NP // 2):
                    pp = slice(2 * p2, 2 * p2 + 2)
                    g1 = gps[0]; g2 = gps[1]
                    for a in range(2):
                        p = 2 * p2 + a
                        rhs = KR[:, p].rearrange("p a t -> p (a t)")
                        P.mm(g1[:, a * 256:(a + 1) * 256], bT[:, p, :], rhs)
                        P.mm(g2[:, a * 256:(a + 1) * 256], kT[:, p, :], rhs)
                    v1 = g1[:].rearrange("p (a b t) -> p a b t", b=2, t=128)
                    v2 = g2[:].rearrange("p (a b t) -> p a b t", b=2, t=128)
                    P.stt(Pm[:, pp, :], v1[:, :, 0, :], -1.0, msk["SU"][:].bc(1, [128, 2, 128]), ALU.mult, ALU.mult)
                    P.tt(Mbr[:, pp, :], v1[:, :, 1, :], msk["UI"][:].bc(1, [128, 2, 128]), ALU.mult)
                    P.tt(Mkk[:, pp, :], v2[:, :, 0, :], msk["SU"][:].bc(1, [128, 2, 128]), ALU.mult)
                    P.tt(Mkr[:, pp, :], v2[:, :, 1, :], msk["UI"][:].bc(1, [128, 2, 128]), ALU.mult)
                for p4 in range(NP // 4):
                    pp = slice(4 * p4, 4 * p4 + 4)
                    i1 = ips[p4 % 2]
                    for a in range(4):
                        p = 4 * p4 + a
                        P.mm(i1[:, a * 128:(a + 1) * 128], KR[:, p, 0, :], bT[:, p, :])
                    P.stt(PTm[:, pp, :], g4(i1), -1.0, msk["SL"][:].bc(1, [128, 4, 128]), ALU.mult, ALU.mult)
                cur, nxt = 0, 1
                P.copy(PTc[cur][:, :, 0, :], Pm[:], eng="pool")
                P.tt(PTc[cur][:, :, 1, :], Pm[:], Ibd[:].bc(1, [128, NP, 128]), ALU.add, eng="pool")
                P.copy(PTt[cur][:], PTm[:], eng="pool")
                for p2 in range(NP // 2):
                    pp = slice(2 * p2, 2 * p2 + 2)
                    i1, i2 = ips[0], ips[1]
                    for a in range(2):
                        p = 2 * p2 + a
                        P.mm(i1[:, a * 128:(a + 1) * 128], PTt[cur][:, p, :], PTc[cur][:, p, 0, :])
                        P.mm(i2[:, a * 128:(a + 1) * 128], PTc[cur][:, p, 0, :], PTt[cur][:, p, :])
                    P.copy(PTc[nxt][:, pp, 0, :], g4(i1)[:, 0:2, :], eng="act")
                    P.copy(PTt[nxt][:, pp, :], g4(i2)[:, 0:2, :], eng="act")
                P.copy(PTc[nxt][:, :, 1, :], PTc[cur][:, :, 1, :], eng="pool")
                cur, nxt = nxt, cur
                for lev in range(1, 6):
                    lastlev = lev == 5
                    for p2 in range(NP // 2):
                        pp = slice(2 * p2, 2 * p2 + 2)
                        i1, i2 = ips[0], ips[1]
                        for a in range(2):
                            p = 2 * p2 + a
                            if lastlev:
                                P.mm(i1[:, a * 256 + 128:(a + 1) * 256], PTt[cur][:, p, :], PTc[cur][:, p, 1, :])
                            else:
                                P.mm(i1[:, a * 256:(a + 1) * 256], PTt[cur][:, p, :],
                                     PTc[cur][:, p].rearrange("p a t -> p (a t)"))
                                P.mm(i2[:, a * 128:(a + 1) * 128], PTc[cur][:, p, 0, :], PTt[cur][:, p, :])
                        v1 = i1[:].rearrange("p (a b t) -> p a b t", b=2, t=128)
                        if lastlev:
                            P.tt(R[:, pp, :], PTc[cur][:, pp, 1, :], v1[:, :, 1, :], ALU.add)
                        else:
                            P.copy(PTc[nxt][:, pp, 0, :], v1[:, :, 0, :], eng="act")
                            P.tt(PTc[nxt][:, pp, 1, :], PTc[cur][:, pp, 1, :], v1[:, :, 1, :], ALU.add)
                            P.copy(PTt[nxt][:, pp, :], g4(i2)[:, 0:2, :], eng="act")
                    cur, nxt = nxt, cur
                xs_ = sps[1]
                for p in range(NP):
                    o = xs_[:, p * 64:(p + 1) * 64]
                    P.mm(o, KR[:, p, 0, :], S0r[:, p, :], start=True, stop=False)
                    P.mm(o, Mkk[:, p, :], V[:, p, :], start=False, stop=True)
                P.copy(Xsb[:], s8(xs_), eng="act")
                us_ = sps[0]
                for p in range(NP):
                    P.mm(us_[:, p * 64:(p + 1) * 64], R[:, p, :], Xsb[:, p, :])
                P.act(nU[:], s8(us_), AF.Copy, scale=-1.0)
                ys_ = sps[1]
                for p in range(NP):
                    o = ys_[:, p * 64:(p + 1) * 64]
                    P.mm(o, KR[:, p, 1, :], S0r[:, p, :], start=True, stop=False)
                    P.mm(o, Mbr[:, p, :], nU[:, p, :], start=False, stop=False)
                    P.mm(o, Mkr[:, p, :], V[:, p, :], start=False, stop=True)
                P.copy(Ysb[:], s8(ys_), eng="dve")
                for hh in range(2):
                    dst = S["y"][rows, c0:c0 + NCOL].rearrange("s (p h k) -> s p h k", h=2, k=64)[:, :, hh, :]
                    P.dma(dst, Ysb[hh * 64:(hh + 1) * 64, :, :], q="sp" if hh else "act")
                if c < NCH - 1:
                    ns_ = sps[0]
                    for p in range(NP):
                        o = ns_[:, p * 64:(p + 1) * 64]
                        P.mm(o, Bh[:, p, :], nU[:, p, :], start=True, stop=False)
                        P.mm(o, Kh[:, p, :], V[:, p, :], start=False, stop=True)
                    P.tt(tmpS[:], S32[:], GLc[:].bc(2, [128, NP, 64]), ALU.mult, eng="pool")
                    P.tt(S32[:], tmpS[:], s8(ns_), ALU.add)
                    P.copy(S0r[:], S32[:], eng="act")


I32 = mybir.dt.int32


def stage_moe_sparse(P, C, x, w_grp, b_grp, w_exp_r, b_exp_r, wgT, wuT, wdT, ffn, T=SEQ):
    ident = C["ident"]
    NT = T // 128
    NB = (2 * T + 32 * 127 + 127) // 128
    NR = NB * 128
    xbuf = dram_scratch(P, "moe_xbuf_%d" % P.nbuf, [NR, D])
    ybuf = dram_scratch(P, "moe_ybuf_%d" % P.nbuf, [NR, D])
    meta = dram_scratch(P, "moe_meta_%d" % P.nbuf, [128, NT, 4])
    widx_d = dram_scratch(P, "moe_widx_%d" % P.nbuf, [128, NB], I32)
    with P.scope():
        wr = P.sb([128, 16, 36])
        P.dma(wr[:, :, 0:4], w_grp[:].rearrange("(kt p) n -> p kt n", p=128))
        P.dma(wr[:, :, 4:36], w_exp_r[:].rearrange("(kt p) n -> p kt n", p=128), q="act")
        br = P.sb([128, 36])
        P.dma(br[:, 0:4], b_grp[:].partition_broadcast(128))
        P.dma(br[:, 4:36], b_exp_r[:].partition_broadcast(128), q="act")
        slt = P.sb([128, 128]); ones = P.sb([128, 128]); iop = P.sb([128, 1]); thr = P.sb([128, NB])
        P.dma(slt[:], C["slt"][:]); P.dma(ones[:], C["ones128"][:], q="act"); P.dma(iop[:], C["iota_p"][:])
        P.dma(thr[:], C["blk_thr"][0:NB].partition_broadcast(128), q="act")
        xin = [P.sb([128, D]) for _ in range(2)]
        xT = [P.sb([128, 16, 128]) for _ in range(2)]
        tps = [P.ps() for _ in range(2)]
        lps = P.ps(); cps = P.ps()
        posA = P.sb([128, NT, 32]); s1A = P.sb([128, NT, 32]); s2A = P.sb([128, NT, 32]); gA = P.sb([128, NT, 2])
        carry = P.sb([128, 32]); P.memset(carry[:], 0.0)
        lg = P.sb([128, 36]); s1 = P.sb([128, 8]); t8 = P.sb([128, 8]); oh = P.sb([128, 4]); junk = P.sb([128, 4])
        lem = P.sb([128, 32]); sel = P.sb([128, 32]); w = P.sb([128, 32]); G = P.sb([128, 32]); t32 = P.sb([128, 32])
        for i in range(NT):
            xi, xt = xin[i % 2], xT[i % 2]
            P.dma(xi[:], x[i * 128:(i + 1) * 128, :], q="sp" if i % 2 else "act")
            for g in range(4):
                tp = tps[g % 2]
                for j in range(4):
                    kt = g * 4 + j
                    P.transpose(tp[:, j * 128:(j + 1) * 128], xi[:, kt * 128:(kt + 1) * 128], ident[:])
                P.copy(xt[:, g * 4:(g + 1) * 4, :], tp[:].rearrange("p (a t) -> p a t", a=4), eng="act" if g % 2 else "dve")
            for kt in range(16):
                P.mm(lps[:, 0:36], xt[:, kt, :], wr[:, kt, :], start=(kt == 0), stop=(kt == 15))
            P.tt(lg[:], lps[:, 0:36], br[:], ALU.add)
            P.reduce(s1[:, 0:1], lg[:, 0:4], ALU.max)
            P.ts(s1[:, 1:2], s1[:, 0:1], -1.0, ALU.mult)
            P.act(junk[:], lg[:, 0:4], AF.Exp, bias=s1[:, 1:2], accum=s1[:, 2:3])
            P.recip(s1[:, 3:4], s1[:, 2:3])
            P.ts(oh[:], lg[:, 0:4], s1[:, 0:1], ALU.is_equal)
            P.ts(oh[:], oh[:], 1e30, ALU.mult, -1e30, ALU.add)
            P.tt(lem[:].rearrange("p (g j) -> p g j", j=8), lg[:, 4:36].rearrange("p (g j) -> p g j", j=8),
                 oh[:].bc(2, [128, 4, 8]), ALU.add)
            P.max8(t8[:], lem[:])
            P.ts(sel[:], lem[:], t8[:, 1:2], ALU.is_ge)
            P.ts(s1A[:, i, :], lem[:], t8[:, 0:1], ALU.is_ge)
            P.tt(s2A[:, i, :], sel[:], s1A[:, i, :], ALU.subtract)
            P.ts(s1[:, 4:5], t8[:, 0:1], -1.0, ALU.mult)
            P.act(w[:], lem[:], AF.Exp, bias=s1[:, 4:5])
            P.act(s1[:, 5:6], t8[:, 1:2], AF.Exp, bias=s1[:, 4:5])
            P.ts(s1[:, 5:6], s1[:, 5:6], 1.0, ALU.add)
            P.recip(s1[:, 6:7], s1[:, 5:6])
            P.tt(s1[:, 7:8], s1[:, 6:7], s1[:, 3:4], ALU.mult)
            P.stt(G[:], w[:], s1[:, 7:8], sel[:], ALU.mult, ALU.mult)
            P.tt(t32[:], G[:], s1A[:, i, :], ALU.mult)
            P.reduce(gA[:, i, 0:1], t32[:], ALU.add)
            P.tt(t32[:], G[:], s2A[:, i, :], ALU.mult)
            P.reduce(gA[:, i, 1:2], t32[:], ALU.add)
            P.mm(cps[:, 0:32], slt[:], sel[:])
            P.tt(posA[:, i, :], cps[:, 0:32], carry[:], ALU.add)
            P.mm(cps[:, 32:64], ones[:], sel[:])
            P.tt(carry[:], carry[:], cps[:, 32:64], ALU.add)
        ci = P.sb([128, 32], I32); padf = P.sb([128, 32]); pend = P.sb([128, 32]); pstart = P.sb([128, 32])
        zero32 = P.sb([128, 32]); P.memset(zero32[:], 0.0); one32 = P.sb([128, 32]); P.memset(one32[:], 1.0)
        P.ts(padf[:], carry[:], 127.0, ALU.add)
        P.copy(ci[:], padf[:])
        P.ts(ci[:], ci[:], 7, ALU.arith_shift_right, 7, ALU.arith_shift_left)
        P.copy(padf[:], ci[:])
        P.scan(pend[:], one32[:], padf[:], 0.0, ALU.mult, ALU.add)
        P.tt(pstart[:], pend[:], padf[:], ALU.subtract)
        dsum = P.sb([128, NT, 32]); d12 = P.sb([128, NT, 2]); d12i = P.sb([128, NT, 2], I32)
        P.tt(dsum[:], posA[:], pstart[:].bc(1, [128, NT, 32]), ALU.add)
        tmpd = P.sb([128, NT, 32])
        P.tt(tmpd[:], dsum[:], s1A[:], ALU.mult)
        P.reduce(d12[:, :, 0], tmpd[:], ALU.add)
        P.tt(tmpd[:], dsum[:], s2A[:], ALU.mult)
        P.reduce(d12[:, :, 1], tmpd[:], ALU.add)
        P.copy(d12i[:], d12[:])
        cmp = P.sb([128, NB, 32]); be = P.sb([128, NB]); wix = P.sb([128, NB], I32)
        P.tt(cmp[:], pend[:].bc(1, [128, NB, 32]), thr[:].bc(2, [128, NB, 32]), ALU.is_le)
        P.reduce(be[:], cmp[:], ALU.add)
        P.ts(be[:], be[:], 31.0, ALU.min)
        same = P.sb([128, NB]); P.memset(same[:], 0.0)
        P.tt(same[:, 1:NB], be[:, 1:NB], be[:, 0:NB - 1], ALU.is_equal)
        P.ts(be[:], be[:], 128.0, ALU.mult, iop[:], ALU.add)
        P.stt(be[:], same[:], 100000.0, be[:], ALU.mult, ALU.add)
        P.copy(wix[:], be[:])
        P.dma(widx_d[:], wix[:])
        mt = P.sb([128, NT, 4])
        P.copy(mt[:, :, 0:2], gA[:])
        P.copy(mt[:, :, 2:4].bitcast(I32), d12i[:])
        P.dma(meta[:], mt[:])
        for i in range(NT):
            xi = xin[i % 2]
            P.dma(xi[:], x[i * 128:(i + 1) * 128, :], q="sp" if i % 2 else "act")
            P.scatter_rows(xbuf[:, :], d12i[:, i, 0:1], xi[:], NR)
            P.scatter_rows(xbuf[:, :], d12i[:, i, 1:2], xi[:], NR)
    with P.scope():
        wix = P.sb([128, NB], I32)
        P.dma(wix[:], widx_d[:])
        Wg = P.sb([128, 16, 512], F32R); Wu = P.sb([128, 16, 512], F32R); Wd = P.sb([128, 4, 2048], F32R)
        xb = [P.sb([128, D]) for _ in range(2)]
        xT = [P.sb([128, 16, 128], F32R) for _ in range(2)]
        hb = [P.sb([128, 512]) for _ in range(2)]; sg = [P.sb([128, 512]) for _ in range(2)]
        hT = [P.sb([128, 4, 128], F32R) for _ in range(2)]
        yb = [P.sb([128, D]) for _ in range(2)]
        tps = [P.ps() for _ in range(2)]
        gp = P.ps(); up = P.ps(); hp = P.ps(); dps = [P.ps() for _ in range(2)]
        for j in range(NB):
            q = j % 2
            ix = wix[:, j:j + 1]
            P.gather_rows(Wg[:].rearrange("p a b -> p (a b)"), wgT[:, :], ix, 4096)
            P.gather_rows(Wu[:].rearrange("p a b -> p (a b)"), wuT[:, :], ix, 4096)
            P.gather_rows(Wd[:].rearrange("p a b -> p (a b)"), wdT[:, :], ix, 4096)
            P.dma(xb[q][:], xbuf[j * 128:(j + 1) * 128, :], q="sp" if q else "act")
            for g in range(4):
                tp = tps[g % 2]
                for jj in range(4):
                    kt = g * 4 + jj
                    P.transpose(tp[:, jj * 128:(jj + 1) * 128], xb[q][:, kt * 128:(kt + 1) * 128], ident[:])
                P.copy(xT[q][:, g * 4:(g + 1) * 4, :], tp[:].rearrange("p (a t) -> p a t", a=4), eng="act" if g % 2 else "dve")
            for kt in range(16):
                P.mm(gp[:], xT[q][:, kt, :], Wg[:, kt, :], start=(kt == 0), stop=(kt == 15))
            for kt in range(16):
                P.mm(up[:], xT[q][:, kt, :], Wu[:, kt, :], start=(kt == 0), stop=(kt == 15))
            P.act(sg[q][:], gp[:], AF.Silu)
            P.tt(hb[q][:], sg[q][:], up[:], ALU.mult)
            for fc in range(4):
                P.transpose(hp[:, fc * 128:(fc + 1) * 128], hb[q][:, fc * 128:(fc + 1) * 128], ident[:])
            P.copy(hT[q][:], hp[:].rearrange("p (a t) -> p a t", a=4), eng="act")
            for dc in range(4):
                dp = dps[dc % 2]
                for fc in range(4):
                    P.mm(dp[:], hT[q][:, fc, :], Wd[:, fc, dc * 512:(dc + 1) * 512], start=(fc == 0), stop=(fc == 3))
                P.copy(yb[q][:, dc * 512:(dc + 1) * 512], dp[:], eng="dve" if dc % 2 else "act")
            P.dma(ybuf[j * 128:(j + 1) * 128, :], yb[q][:], q="sp" if q else "act")
    with P.scope():
        mt = P.sb([128, NT, 4])
        P.dma(mt[:], meta[:])
        y1 = [P.sb([128, D]) for _ in range(2)]; y2 = [P.sb([128, D]) for _ in range(2)]
        for i in range(NT):
            q = i % 2
            P.gather_rows(y1[q][:], ybuf[:, :], mt[:, i, 2:3].bitcast(I32), NR)
            P.gather_rows(y2[q][:], ybuf[:, :], mt[:, i, 3:4].bitcast(I32), NR)
            P.ts(y1[q][:], y1[q][:], mt[:, i, 0:1], ALU.mult)
            P.stt(y1[q][:], y2[q][:], mt[:, i, 1:2], y1[q][:], ALU.mult, ALU.add)
            P.dma(ffn[i * 128:(i + 1) * 128, :], y1[q][:], q="sp" if q else "act")
```
